# Optimizing a Trainium2 kernel written in Bass

```python
import functools
import jax
import jax.numpy as jnp
from jax import lax
import numpy as np

D_MODEL = 1024
BATCH = 4
SEQ = 4096
DEPTH = 4

GRID_W = 64
CTX_LEN = 256
EPS = 1e-6

HG_HEADS = 4
HG_DK = 128
HG_DV = 128
HG_CHUNK = 64
HG_W = HG_HEADS * HG_DK
HG_OUT = HG_HEADS * HG_DV

MLA_HEADS = 4
MLA_Q_RANK = 256
MLA_KV_RANK = 128
MLA_NOPE = 128
MLA_ROPE = 64
MLA_V = 128
MLA_OUT = MLA_HEADS * MLA_V
MLA_SCALE = (MLA_NOPE + MLA_ROPE) ** -0.5
ROPE_THETA = 10000.0
Q_BLOCK = 128

IN_SIZES = (HG_W, HG_W, HG_W, HG_OUT, HG_OUT, MLA_Q_RANK, MLA_KV_RANK, MLA_ROPE)
IN_WIDTH = 3008
MIX_OUT = HG_OUT + MLA_OUT

POOL_WINDOWS = (2, 4, 8, 16)
POOL_GROUP = D_MODEL // 4

N_EXPERTS = 16
EC_FACTOR = 2
EXPERT_FF = 2048

N_EVEN = (DEPTH + 1) // 2
N_ODD = DEPTH // 2

kernel_name = 'hybrid_flow_trunk'


def rms_norm(x, g):
    xf = x.astype(jnp.float32)
    y = xf * lax.rsqrt(jnp.mean(xf * xf, axis=-1, keepdims=True) + EPS)
    return (y * g.astype(jnp.float32)).astype(x.dtype)


def modulate(x, g, shift, scale):
    return rms_norm(x, g) * (1 + scale) + shift


def to_heads(t, n_heads):
    b, n, _ = t.shape
    return t.reshape(b, n, n_heads, -1).transpose(0, 2, 1, 3)


def from_heads(t):
    b, h, n, d = t.shape
    return t.transpose(0, 2, 1, 3).reshape(b, n, h * d)


def hg_heads(t):
    return to_heads(t, HG_HEADS).astype(jnp.float32)


def maybe_flip(t, rev):
    return jnp.flip(t, axis=2) if rev else t


def axial_rope(n_tok):
    rows = n_tok // GRID_W
    row = jnp.repeat(jnp.arange(rows, dtype=jnp.float32), GRID_W)
    col = jnp.tile(jnp.arange(GRID_W, dtype=jnp.float32), rows)
    half = MLA_ROPE // 2
    inv = 1.0 / (ROPE_THETA ** (jnp.arange(0, half, 2, dtype=jnp.float32) / half))
    ar = row[:, None] * inv[None, :]
    ac = col[:, None] * inv[None, :]
    ang = jnp.concatenate([ar, ar, ac, ac], axis=-1)
    return jnp.cos(ang), jnp.sin(ang)


def apply_rope(x, cos, sin):
    x0, x1, x2, x3 = jnp.split(x, 4, axis=-1)
    rot = jnp.concatenate([-x1, x0, -x3, x2], axis=-1)
    return (x.astype(jnp.float32) * cos + rot.astype(jnp.float32) * sin).astype(x.dtype)


def hgrn_gates(z, lb):
    lb = lb[None, :, None, :]
    logf = jnp.logaddexp(jnp.log(lb), jnp.log1p(-lb) + jax.nn.log_sigmoid(z))
    k = (1.0 - lb) * jax.nn.sigmoid(-z)
    return logf, k


def chunk_scan(q, k, v, logf, s0):
    b, h, n, _ = q.shape
    dv = v.shape[-1]
    nc = n // HG_CHUNK

    def to_chunks(t):
        return jnp.moveaxis(t.reshape(b, h, nc, HG_CHUNK, t.shape[-1]), 2, 0)

    tri = jnp.tril(jnp.ones((HG_CHUNK, HG_CHUNK), dtype=bool))[:, :, None]

    def step(s, inp):
        qc, kc, vc, gc = inp
        cb = jnp.cumsum(gc, axis=2)
        o_inter = jnp.einsum('bhtk,bhkv->bhtv', qc * jnp.exp(cb), s)
        diff = cb[:, :, :, None, :] - cb[:, :, None, :, :]
        dec = jnp.exp(jnp.where(tri, diff, -jnp.inf))
        att = jnp.einsum('bhtk,bhsk,bhtsk->bhts', qc, kc, dec)
        o = o_inter + jnp.einsum('bhts,bhsv->bhtv', att, vc)
        cb_last = cb[:, :, -1:, :]
        s_new = jnp.exp(cb_last[:, :, 0, :, None]) * s + jnp.einsum('bhsk,bhsv->bhkv', kc * jnp.exp(cb_last - cb), vc)
        return s_new, o

    s_fin, o = lax.scan(step, s0, (to_chunks(q), to_chunks(k), to_chunks(v), to_chunks(logf)))
    return jnp.moveaxis(o, 0, 2).reshape(b, h, n, dv), s_fin


def final_state(k, v, logf):
    g = jnp.cumsum(logf, axis=2)
    w = jnp.exp(g[:, :, -1:, :] - g)
    return jnp.einsum('bhnk,bhnv->bhkv', k * w, v)


def hgrn_readout(o, g, hg_g, dtype):
    gate = jax.nn.silu(to_heads(g, HG_HEADS).astype(jnp.float32))
    return from_heads(rms_norm(o, hg_g) * gate).astype(dtype)


def mla_q(qa, qn_g, wq_b):
    b, n, _ = qa.shape
    q = (rms_norm(qa, qn_g) @ wq_b).reshape(b, n, MLA_HEADS, MLA_NOPE + MLA_ROPE)
    return q[..., :MLA_NOPE], q[..., MLA_NOPE:]


def mla_kv(kva, kvn_g, wkv_b):
    b, n, _ = kva.shape
    kv = (rms_norm(kva, kvn_g) @ wkv_b).reshape(b, n, MLA_HEADS, MLA_NOPE + MLA_V)
    return kv[..., :MLA_NOPE], kv[..., MLA_NOPE:]


def block_attention(q_nope, q_pe, k_nope, k_pe, v):
    b, n, h, _ = q_nope.shape
    nb = n // Q_BLOCK

    def blocks(t):
        return jnp.moveaxis(t.reshape(b, nb, Q_BLOCK, h, t.shape[-1]), 1, 0)

    def one_block(args):
        qn, qp = args
        s = jnp.einsum('bqhd,bkhd->bhqk', qn, k_nope) + jnp.einsum('bqhr,bkr->bhqk', qp, k_pe)
        p = jax.nn.softmax(s.astype(jnp.float32) * MLA_SCALE, axis=-1)
        return jnp.einsum('bhqk,bkhd->bqhd', p.astype(v.dtype), v)

    o = lax.map(one_block, (blocks(q_nope), blocks(q_pe)))
    return jnp.moveaxis(o, 0, 1).reshape(b, n, h * v.shape[-1])


def even_mixer(h_lat, h_ctx, w_in, lb, hg_g, qn_g, wq_b, kvn_g, wkv_b, w_out, cos, sin, ctx_out):
    offs = np.cumsum(IN_SIZES)[:-1].tolist()
    q_l, ffw_l, fbw_l, i_l, g_l, qa_l, kva_l, kpe_l = jnp.split(h_lat @ w_in, offs, axis=-1)
    q_c, ffw_c, fbw_c, i_c, g_c, qa_c, kva_c, kpe_c = jnp.split(h_ctx @ w_in, offs, axis=-1)
    ql, vl, vc = hg_heads(q_l), hg_heads(i_l), hg_heads(i_c)
    qc = hg_heads(q_c) if ctx_out else None
    s0 = jnp.zeros((h_lat.shape[0], HG_HEADS, HG_DK, HG_DV), jnp.float32)
    lat_o, ctx_o = [], []
    for d, (z_l, z_c) in enumerate(((ffw_l, ffw_c), (fbw_l, fbw_c))):
        rev = d == 1
        lb_d = lb[d].reshape(HG_HEADS, HG_DK)
        logf_l, k_l = hgrn_gates(hg_heads(z_l), lb_d)
        logf_c, k_c = hgrn_gates(hg_heads(z_c), lb_d)
        if ctx_out:
            o_c, s_c = chunk_scan(maybe_flip(qc, rev), maybe_flip(k_c, rev), maybe_flip(vc, rev), maybe_flip(logf_c, rev), s0)
            ctx_o.append(maybe_flip(o_c, rev))
        else:
            s_c = final_state(maybe_flip(k_c, rev), maybe_flip(vc, rev), maybe_flip(logf_c, rev))
        o_l, _ = chunk_scan(maybe_flip(ql, rev), maybe_flip(k_l, rev), maybe_flip(vl, rev), maybe_flip(logf_l, rev), s_c)
        lat_o.append(maybe_flip(o_l, rev))
    hg_lat = hgrn_readout(lat_o[0] + lat_o[1], g_l, hg_g, h_lat.dtype)
    kn_l, v_l = mla_kv(kva_l, kvn_g, wkv_b)
    kn_c, v_c = mla_kv(kva_c, kvn_g, wkv_b)
    qn_l, qp_l = mla_q(qa_l, qn_g, wq_b)
    qp_l = apply_rope(qp_l, cos[:, None, :], sin[:, None, :])
    kp_l = apply_rope(kpe_l, cos, sin)
    mla_lat = block_attention(qn_l, qp_l,
                              jnp.concatenate([kn_c, kn_l], axis=1),
                              jnp.concatenate([kpe_c, kp_l], axis=1),
                              jnp.concatenate([v_c, v_l], axis=1))
    y_lat = jnp.concatenate([hg_lat, mla_lat], axis=-1) @ w_out
    if not ctx_out:
        return y_lat, None
    hg_ctx = hgrn_readout(ctx_o[0] + ctx_o[1], g_c, hg_g, h_ctx.dtype)
    qn_c, qp_c = mla_q(qa_c, qn_g, wq_b)
    mla_ctx = block_attention(qn_c, qp_c, kn_c, kpe_c, v_c)
    y_ctx = jnp.concatenate([hg_ctx, mla_ctx], axis=-1) @ w_out
    return y_lat, y_ctx


def pool_mixer(h, w_pool, scale):
    b, n, d = h.shape
    hg = h.astype(jnp.float32).reshape(b, n, len(POOL_WINDOWS), POOL_GROUP)
    cs = jnp.concatenate([jnp.zeros((b, 1, len(POOL_WINDOWS), POOL_GROUP), jnp.float32), jnp.cumsum(hg, axis=1)], axis=1)
    t = jnp.arange(n)
    pooled = []
    for gi, w in enumerate(POOL_WINDOWS):
        lo = jnp.clip(t - w // 2, 0, n - 1)
        hi = jnp.clip(t + w // 2 - 1, 0, n - 1)
        csg = cs[:, :, gi]
        mean = (csg[:, hi + 1] - csg[:, lo]) / (hi - lo + 1).astype(jnp.float32)[None, :, None]
        pooled.append(mean - hg[:, :, gi])
    y = jnp.einsum('bngc,gcd->bngd', jnp.stack(pooled, axis=2), w_pool.astype(jnp.float32))
    return (y.reshape(b, n, d) * scale.astype(jnp.float32)).astype(h.dtype)


def ec_moe(h, router_w, wg, wu, wd):
    b, n, d = h.shape
    cap = EC_FACTOR * n // N_EXPERTS
    aff = jax.nn.softmax(jnp.einsum('bnd,de->bne', h, router_w).astype(jnp.float32), axis=-1)
    gate, idx = lax.top_k(jnp.swapaxes(aff, 1, 2), cap)
    xs = jax.vmap(lambda hb, ib: hb[ib])(h, idx)
    hid = jax.nn.silu(jnp.einsum('becd,edf->becf', xs, wg)) * jnp.einsum('becd,edf->becf', xs, wu)
    out = jnp.einsum('becf,efd->becd', hid, wd) * gate[..., None].astype(h.dtype)
    return jax.vmap(lambda ob, ib: jax.ops.segment_sum(ob.reshape(-1, d), ib.reshape(-1), num_segments=n))(out, idx)


def setup_inputs(seed: int = 0) -> dict:
    key = jax.random.key(seed)
    ks = jax.random.split(key, 23)
    D = D_MODEL

    def nrm(k, shape, s):
        return jax.random.normal(k, shape, jnp.float32) * s

    return {
        'x': nrm(ks[0], (BATCH, SEQ, D), 1.0),
        'c': nrm(ks[1], (BATCH, D), 1.0),
        'ctx': nrm(ks[2], (BATCH, CTX_LEN, D), 1.0),
        'c_ctx': nrm(ks[3], (D,), 1.0),
        'ada_w': nrm(ks[4], (DEPTH, D, 6 * D), 0.5 * D ** -0.5),
        'ada_b': nrm(ks[5], (DEPTH, 6 * D), 0.02),
        'norm1_g': 1.0 + nrm(ks[6], (DEPTH, D), 0.05),
        'norm2_g': 1.0 + nrm(ks[7], (DEPTH, D), 0.05),
        'w_in': nrm(ks[8], (N_EVEN, D, IN_WIDTH), D ** -0.5),
        'hg_lb': nrm(ks[9], (N_EVEN, 2, HG_W), 0.1),
        'hg_norm_g': 1.0 + nrm(ks[10], (N_EVEN, HG_DV), 0.05),
        'mla_qn_g': 1.0 + nrm(ks[11], (N_EVEN, MLA_Q_RANK), 0.05),
        'mla_wq_b': nrm(ks[12], (N_EVEN, MLA_Q_RANK, MLA_HEADS * (MLA_NOPE + MLA_ROPE)), MLA_Q_RANK ** -0.5),
        'mla_kvn_g': 1.0 + nrm(ks[13], (N_EVEN, MLA_KV_RANK), 0.05),
        'mla_wkv_b': nrm(ks[14], (N_EVEN, MLA_KV_RANK, MLA_HEADS * (MLA_NOPE + MLA_V)), MLA_KV_RANK ** -0.5),
        'w_out': nrm(ks[15], (N_EVEN, MIX_OUT, D), MIX_OUT ** -0.5),
        'pool_w': nrm(ks[16], (N_ODD, len(POOL_WINDOWS), POOL_GROUP, POOL_GROUP), POOL_GROUP ** -0.5),
        'pool_scale': 1.0 + nrm(ks[17], (N_ODD, D), 0.1),
        'router_w': nrm(ks[18], (DEPTH, D, N_EXPERTS), D ** -0.5),
        'exp_wg': nrm(ks[19], (DEPTH, N_EXPERTS, D, EXPERT_FF), D ** -0.5),
        'exp_wu': nrm(ks[20], (DEPTH, N_EXPERTS, D, EXPERT_FF), D ** -0.5),
        'exp_wd': nrm(ks[21], (DEPTH, N_EXPERTS, EXPERT_FF, D), EXPERT_FF ** -0.5),
        'final_g': 1.0 + nrm(ks[22], (D,), 0.05),
    }


def reference(x, c, ctx, c_ctx, ada_w, ada_b, norm1_g, norm2_g, w_in, hg_lb, hg_norm_g,
              mla_qn_g, mla_wq_b, mla_kvn_g, mla_wkv_b, w_out, pool_w, pool_scale,
              router_w, exp_wg, exp_wu, exp_wd, final_g):
    cos, sin = axial_rope(x.shape[1])
    lb_all = jnp.cumsum(jax.nn.softmax(hg_lb.astype(jnp.float32), axis=0), axis=0)
    lb_all = lb_all - lb_all[:1]
    last_reader = 2 * (N_EVEN - 1)
    x_lat, x_ctx = x, ctx
    for l in range(DEPTH):
        j = l // 2
        ctx_in = l <= last_reader
        ctx_out = l < last_reader
        mod = [m[:, None, :] for m in jnp.split(jax.nn.silu(c) @ ada_w[l] + ada_b[l], 6, axis=-1)]
        h_lat = modulate(x_lat, norm1_g[l], mod[0], mod[1])
        if ctx_in:
            mod_c = jnp.split(jax.nn.silu(c_ctx) @ ada_w[l] + ada_b[l], 6, axis=-1)
            h_ctx = modulate(x_ctx, norm1_g[l], mod_c[0], mod_c[1])
        if l % 2 == 0:
            y_lat, y_ctx = even_mixer(h_lat, h_ctx, w_in[j], lb_all[j], hg_norm_g[j], mla_qn_g[j], mla_wq_b[j],
                                      mla_kvn_g[j], mla_wkv_b[j], w_out[j], cos, sin, ctx_out)
        else:
            y_lat = pool_mixer(h_lat, pool_w[j], pool_scale[j])
            y_ctx = pool_mixer(h_ctx, pool_w[j], pool_scale[j]) if ctx_out else None
        x_lat = x_lat + mod[2] * y_lat
        h2 = modulate(x_lat, norm2_g[l], mod[3], mod[4])
        x_lat = x_lat + mod[5] * ec_moe(h2, router_w[l], exp_wg[l], exp_wu[l], exp_wd[l])
        if ctx_out:
            x_ctx = x_ctx + mod_c[2] * y_ctx
            h2c = modulate(x_ctx, norm2_g[l], mod_c[3], mod_c[4])
            x_ctx = x_ctx + mod_c[5] * ec_moe(h2c, router_w[l], exp_wg[l], exp_wu[l], exp_wd[l])
    return rms_norm(x_lat, final_g)
```

```python
import numpy as np
import ml_dtypes
from contextlib import ExitStack
import concourse.bass as bass
import concourse.mybir as mybir
from concourse.bass_utils import run_bass_kernel_spmd

F32 = mybir.dt.float32
BF16 = mybir.dt.bfloat16
I32 = mybir.dt.int32
AF = mybir.ActivationFunctionType
ALU = mybir.AluOpType
AX = mybir.AxisListType


class V:
    __slots__ = ("buf", "ap")

    def __init__(self, buf, ap):
        self.buf = buf
        self.ap = ap

    def __getitem__(self, idx):
        return V(self.buf, self.ap[idx])

    def re(self, pat, **kw):
        return V(self.buf, self.ap.rearrange(pat, **kw))

    def bc(self, shape):
        return V(self.buf, self.ap.to_broadcast(list(shape)))

    def pbc(self, n):
        return V(self.buf, self.ap.partition_broadcast(n))

    def us(self, axis):
        return V(self.buf, self.ap.unsqueeze(axis))

    def bitcast(self, dt):
        return V(self.buf, self.ap.bitcast(dt))


class Buf:
    def __init__(self, name, t, space):
        self.name = name
        self.t = t
        self.space = space
        self.lw = None
        self.rd = {}
        self.chan = None

    def __getitem__(self, idx):
        return V(self, self.t[idx])

    @property
    def v(self):
        return V(self, self.t[:] if self.space != "dram" else self.t)


class Chan:
    def __init__(self):
        self.sem = None
        self.cnt = 0


class Sched:
    SEM_MAX = 30000
    NCHAN = 20

    def __init__(self, nc):
        self.nc = nc
        self.E = {"pe": nc.tensor, "dve": nc.vector, "act": nc.scalar,
                  "pool": nc.gpsimd, "sp": nc.sync}
        self.sems = {}
        self.esem = {}
        self.ecnt = {}
        self.retired = []
        self.nsem = 0
        for k in self.E:
            self._new_esem(k)
        self.waited = {k: {} for k in self.E}
        self.chans = [Chan() for _ in range(self.NCHAN)]
        self.chan_rr = 0
        self.stacks = []
        self.ninst = 0

    def _alloc_sem(self, name):
        self.nsem += 1
        nm = "%s_%d" % (name, self.nsem)
        self.sems[nm] = self.nc.alloc_semaphore(name=nm)
        return nm

    def _new_esem(self, k):
        if k in self.esem and self.ecnt[k] > 0:
            self.retired.append((self.esem[k], self.ecnt[k]))
        self.esem[k] = self._alloc_sem("e" + k)
        self.ecnt[k] = 0

    def _wait(self, e, toks):
        need = {}
        for (n, v) in toks:
            if v > need.get(n, 0):
                need[n] = v
        w = self.waited[e]
        for n, v in need.items():
            if w.get(n, 0) >= v:
                continue
            self.E[e].wait_ge(self.sems[n], v)
            w[n] = v

    def push(self):
        self.stacks.append(ExitStack())

    def pop(self):
        self.barrier()
        self.stacks.pop().close()

    def sb(self, name, shape, dtype):
        self.nsem += 0
        self.uid = getattr(self, "uid", 0) + 1
        name = "%s_u%d" % (name, self.uid)
        t = self.stacks[-1].enter_context(self.nc.sbuf_tensor(name, list(shape), dtype))
        return Buf(name, t, "sbuf")

    def ps(self, name, shape, dtype=F32):
        self.uid = getattr(self, "uid", 0) + 1
        name = "%s_u%d" % (name, self.uid)
        t = self.stacks[-1].enter_context(self.nc.psum_tensor(name, list(shape), dtype))
        return Buf(name, t, "psum")

    def dram(self, name, shape, dtype, kind="Internal"):
        t = self.nc.dram_tensor(name, list(shape), dtype, kind=kind).ap()
        return Buf(name, t, "dram")

    def op(self, e, fn, reads=(), writes=()):
        own = self.esem[e]
        deps = []
        for x in reads:
            b = x.buf if isinstance(x, V) else x
            if b.lw is not None:
                deps.append(b.lw)
        for x in writes:
            b = x.buf if isinstance(x, V) else x
            if b.lw is not None and (b.lw[0] != own or e == "pool"):
                deps.append(b.lw)
            for n, v in b.rd.items():
                if n != own or e == "pool":
                    deps.append((n, v))
        self._wait(e, deps)
        inst = fn(self.E[e])
        if self.ecnt[e] >= self.SEM_MAX:
            self._new_esem(e)
        self.ecnt[e] += 1
        inst.then_inc(self.sems[self.esem[e]], 1)
        tok = (self.esem[e], self.ecnt[e])
        for x in reads:
            b = x.buf if isinstance(x, V) else x
            b.rd[tok[0]] = tok[1]
        for x in writes:
            b = x.buf if isinstance(x, V) else x
            b.lw = tok
            b.rd = {}
        self.ninst += 1
        return inst

    def _chan_of(self, b):
        if b.chan is None:
            b.chan = self.chans[self.chan_rr % self.NCHAN]
            self.chan_rr += 1
        return b.chan

    def dma(self, q, out, in_, owner=None, indirect=None, **kw):
        ob, ib = out.buf, in_.buf
        if owner is None:
            owner = ob if ob.space == "sbuf" else ib
        ch = self._chan_of(owner)
        if ch.sem is None or ch.cnt + 16 > self.SEM_MAX:
            if ch.sem is not None:
                self.retired.append((ch.sem, ch.cnt))
                self._wait(q, [(ch.sem, ch.cnt)])
            ch.sem = self._alloc_sem("d")
            ch.cnt = 0
        deps = []
        if ib.lw is not None:
            deps.append(ib.lw)
        if ob.lw is not None:
            deps.append(ob.lw)
        deps += list(ob.rd.items())
        extra_reads = kw.pop("extra_reads", ())
        for x in extra_reads:
            if x.buf.lw is not None:
                deps.append(x.buf.lw)
        if ch.cnt > 0:
            deps.append((ch.sem, ch.cnt))
        self._wait(q, deps)
        if indirect is None:
            inst = self.E[q].dma_start(out=out.ap, in_=in_.ap, **kw)
        else:
            inst = indirect(self.E[q])
        ch.cnt += 16
        inst.then_inc(self.sems[ch.sem], 16)
        tok = (ch.sem, ch.cnt)
        ob.lw = tok
        ob.rd = {}
        ib.rd[tok[0]] = tok[1]
        for x in extra_reads:
            x.buf.rd[tok[0]] = tok[1]
        self.ninst += 1
        return inst

    def barrier(self):
        toks = [(self.esem[k], self.ecnt[k]) for k in self.E if self.ecnt[k] > 0]
        toks += [(c.sem, c.cnt) for c in self.chans if c.sem is not None and c.cnt > 0]
        toks += self.retired
        for e in self.E:
            self._wait(e, toks)
        self.retired = []

    def mm(self, out, lhsT, rhs, start=True, stop=True):
        return self.op("pe", lambda e: e.matmul(out.ap, lhsT.ap, rhs.ap, start=start, stop=stop),
                       reads=(lhsT, rhs), writes=(out,))

    def tr(self, out, in_, ident):
        return self.op("pe", lambda e: e.transpose(out.ap, in_.ap, ident.ap),
                       reads=(in_, ident), writes=(out,))

    def act(self, out, in_, func, bias=None, scale=None, accum=None, eng="act"):
        kw = {}
        rd = [in_]
        wr = [out]
        if bias is not None:
            if isinstance(bias, V):
                kw["bias"] = bias.ap
                rd.append(bias)
            else:
                kw["bias"] = bias
        if scale is not None:
            if isinstance(scale, V):
                kw["scale"] = scale.ap
                rd.append(scale)
            else:
                kw["scale"] = scale
        if accum is not None:
            kw["accum_out"] = accum.ap
            wr.append(accum)
        return self.op("act", lambda e: e.activation(out.ap, in_.ap, func, **kw), reads=rd, writes=wr)

    def tt(self, out, a, b, op, eng="dve"):
        return self.op(eng, lambda e: e.tensor_tensor(out.ap, a.ap, b.ap, op), reads=(a, b), writes=(out,))

    def ts(self, out, a, s1, op0, s2=None, op1=None, accum=None, eng="dve"):
        rd = [a]
        wr = [out]
        a1 = s1
        a2 = s2
        if isinstance(s1, V):
            rd.append(s1)
            a1 = s1.ap
        if isinstance(s2, V):
            rd.append(s2)
            a2 = s2.ap
        kw = {}
        if op1 is not None:
            kw["op1"] = op1
        if accum is not None:
            kw["accum_out"] = accum.ap
            wr.append(accum)
        return self.op(eng, lambda e: e.tensor_scalar(out.ap, a.ap, a1, a2, op0, **kw), reads=rd, writes=wr)

    def stt(self, out, a, s, b, op0, op1):
        rd = [a, b]
        sv = s
        if isinstance(s, V):
            rd.append(s)
            sv = s.ap
        return self.op("dve", lambda e: e.scalar_tensor_tensor(out.ap, a.ap, sv, b.ap, op0, op1),
                       reads=rd, writes=(out,))

    def cp(self, out, in_, eng="dve"):
        if eng == "act":
            return self.op("act", lambda e: e.copy(out.ap, in_.ap), reads=(in_,), writes=(out,))
        return self.op(eng, lambda e: e.tensor_copy(out.ap, in_.ap), reads=(in_,), writes=(out,))

    def memset(self, out, val, eng="dve"):
        return self.op(eng, lambda e: e.memset(out.ap, val), writes=(out,))

    def red(self, out, in_, op, axis=AX.X):
        return self.op("dve", lambda e: e.tensor_reduce(out.ap, in_.ap, axis, op), reads=(in_,), writes=(out,))

    def recip(self, out, in_):
        return self.op("dve", lambda e: e.reciprocal(out.ap, in_.ap), reads=(in_,), writes=(out,))


D = 1024
NT = 34
NTOK = NT * 128
NLAT = 4096
NCTX = 256
EPS = 1e-6
NE = 16
FF = 2048
POOL_WINDOWS = (2, 4, 8, 16)
MLA_SCALE = (128 + 64) ** -0.5
O_Q, O_FFW, O_FBW, O_I, O_G, O_QA, O_KVA, O_KPE = 0, 512, 1024, 1536, 2048, 2560, 2816, 2944


class NS:
    pass


def host_consts():
    c = {}
    c["identf"] = np.eye(128, dtype=np.float32)
    c["onesf"] = np.ones((128, 128), np.float32)
    p = np.arange(128)
    c["lstrict"] = (p[:, None] < p[None, :]).astype(np.float32)
    c["iota"] = np.broadcast_to(np.arange(512, dtype=np.float32), (128, 512)).copy()
    c["tokid"] = (np.arange(NT)[None, :] * 128 + p[:, None]).astype(np.float32)
    c["toka"] = np.floor(c["tokid"] / 64.0).astype(np.float32)
    c["tokb"] = (c["tokid"] - 64.0 * c["toka"]).astype(np.float32)
    s = (p % 64)[:, None]
    t = np.arange(64)[None, :]
    c["trimask"] = np.stack([(s <= t), (s >= t)], axis=1).astype(np.float32)
    rm = np.ones((128, 512), np.float32)
    rm[:, ::64] = 0.0
    c["rmask"] = rm
    band = np.zeros((4, 5, 128, 128), np.float32)
    n = 3 * 128
    for gi, w in enumerate(POOL_WINDOWS):
        def full(nseq, tile):
            tt_ = np.arange(nseq)
            lo = np.clip(tt_ - w // 2, 0, nseq - 1)
            hi = np.clip(tt_ + w // 2 - 1, 0, nseq - 1)
            M = np.zeros((nseq, nseq), np.float64)
            for ti in range(nseq):
                M[lo[ti]:hi[ti] + 1, ti] = 1.0 / (hi[ti] - lo[ti] + 1)
                M[ti, ti] -= 1.0
            return M
        M = full(n, 1)
        band[gi, 0] = M[0:128, 128:256]
        band[gi, 1] = M[128:256, 128:256]
        band[gi, 2] = M[256:384, 128:256]
        band[gi, 3] = M[0:128, 0:128]
        band[gi, 4] = M[256:384, 256:384]
    c["band"] = np.ascontiguousarray(band.transpose(2, 0, 1, 3)).reshape(128, 20 * 128)
    rows = NLAT // 64
    row = np.repeat(np.arange(rows, dtype=np.float32), 64)
    col = np.tile(np.arange(64, dtype=np.float32), rows)
    half = 32
    inv = (1.0 / (np.float32(10000.0) ** (np.arange(0, half, 2, dtype=np.float32) / np.float32(half)))).astype(np.float32)
    ar = row[:, None] * inv[None, :]
    ac = col[:, None] * inv[None, :]
    ang = np.concatenate([ar, ar, ac, ac], axis=-1).astype(np.float32)
    cos = np.ones((64, NTOK), np.float32)
    sin = np.zeros((64, NTOK), np.float32)
    cos[:, NCTX:] = np.cos(ang).T
    sin[:, NCTX:] = np.sin(ang).T
    c["cosT"] = cos
    c["sinT"] = sin
    return c


CONST_SHAPES = {"identf": [128, 128], "onesf": [128, 128], "lstrict": [128, 128], "iota": [128, 512],
                "tokid": [128, NT], "toka": [128, NT], "tokb": [128, NT], "trimask": [128, 2, 64], "rmask": [128, 512], "band": [128, 2560],
                "cosT": [64, NTOK], "sinT": [64, NTOK]}

INPUT_SHAPES = {
    "x": [NLAT, D], "ctx": [NCTX, D], "c_fm": [128, 8], "cctx_fm": [128, 8],
    "ada_w": [4, D, 6 * D], "ada_b": [4, 6 * D], "norm1_g": [4, D], "norm2_g": [4, D],
    "w_in": [2, D, 3008], "hg_lb_fm": [128, 16], "hg_norm_g": [2, 128], "mla_qn_g_fm": [128, 4],
    "mla_wq_b": [2, 256, 768], "mla_kvn_g_fm": [128, 2], "mla_wkv_b": [2, 128, 1024], "w_out": [2, D, D],
    "pool_w": [2, 4, 256, 256], "pool_scale": [2, D], "router_w": [4, D, NE],
    "exp_wg": [4, NE, D, FF], "exp_wu": [4, NE, D, FF], "exp_wd": [4, NE, FF, D], "final_g": [1, D],
}


def setup(S, debug_out=()):
    C = NS()
    for k, shp in INPUT_SHAPES.items():
        setattr(C, k, S.dram(k, shp, F32, kind="ExternalInput"))
    for k, shp in CONST_SHAPES.items():
        setattr(C, "h_" + k, S.dram("k_" + k, shp, F32, kind="ExternalInput"))
    C.out = S.dram("out", [NLAT, D], F32, kind="ExternalOutput")
    C.xd = S.dram("xd", [NTOK, D], F32, kind="ExternalOutput" if "xd" in debug_out else "Internal")
    kd = lambda n: "ExternalOutput" if n in debug_out else "Internal"
    C.h2d = S.dram("h2d", [NTOK, D], BF16, kind=kd("h2d"))
    C.mixd = S.dram("mixd", [NTOK, D], BF16, kind=kd("mixd"))
    C.modtab = S.dram("modtab", [4, 2, 6, D], F32)
    C.knT_d = S.dram("knT_d", [128, 4, NTOK], BF16, kind=kd("knT_d"))
    C.kpeT_d = S.dram("kpeT_d", [64, NTOK], BF16, kind=kd("kpeT_d"))
    C.v_d = S.dram("v_d", [128, NT, 4, 130], BF16, kind=kd("v_d"))
    C.qnT_d = S.dram("qnT_d", [128, 2, NTOK], BF16, kind=kd("qnT_d"))
    C.qk_d = S.dram("qk_d", [2, 128, 4, 2, NTOK], BF16)
    C.khT_d = S.dram("khT_d", [2, 128, NT, 4, 128], BF16)
    C.vh_d = S.dram("vh_d", [128, NT, 512], BF16)
    C.sg_d = S.dram("sg_d", [128, NT, 512], BF16)
    C.identf = S.sb("identf", [128, 128], F32)
    C.identb = S.sb("identb", [128, 128], BF16)
    C.onesf = S.sb("onesf", [128, 128], F32)
    C.epsb = S.sb("epsb", [128, 1], F32)
    S.dma("sp", C.identf.v, C.h_identf.v)
    S.dma("sp", C.onesf.v, C.h_onesf.v)
    S.cp(C.identb.v, C.identf.v)
    S.memset(C.epsb.v, EPS)
    S.dma("sp", C.xd.v[0:NCTX, :], C.ctx.v, owner=C.identf)
    S.dma("sp", C.xd.v[NCTX:NTOK, :], C.x.v, owner=C.onesf)
    return C


def norm_mod(S, C, xt, A, Bv, out, W):
    S.act(W.junk.v, xt, AF.Square, accum=W.ssq.v)
    S.act(W.lnv.v, W.ssq.v, AF.Ln, scale=1.0 / D, bias=C.epsb.v)
    S.act(W.rstd.v, W.lnv.v, AF.Exp, scale=-0.5)
    S.stt(W.tmp.v, xt, W.rstd.v, A, ALU.mult, ALU.mult)
    S.tt(out, W.tmp.v, Bv, ALU.add)


def norm_ws(S, pfx):
    W = NS()
    W.junk = S.sb(pfx + "junk", [128, D], F32)
    W.tmp = S.sb(pfx + "tmp", [128, D], F32)
    W.ssq = S.sb(pfx + "ssq", [128, 1], F32)
    W.rstd = S.sb(pfx + "rstd", [128, 1], F32)
    W.lnv = S.sb(pfx + "lnv", [128, 1], F32)
    return W


def prologue(S, C, layers):
    S.push()
    cin = S.sb("cin", [128, 2, 8], F32)
    sc = S.sb("sc", [128, 2, 8], F32)
    S.dma("sp", cin[:, 0, :], C.c_fm.v)
    S.dma("sp", cin[:, 1, :], C.cctx_fm.v)
    S.act(sc.v, cin.v, AF.Silu)
    lhs2 = S.sb("lhs2", [128, 8, 2], F32)
    S.cp(lhs2.v, sc.v.re("p a k -> p k a"))
    g1 = S.sb("g1", [2, D], F32)
    g2 = S.sb("g2", [2, D], F32)
    psc = S.sb("psc", [2, D], F32)
    wb = [S.sb("adaw%d" % i, [128, 8, D], F32) for i in range(3)]
    brow = [S.sb("brow%d" % i, [1, D], F32) for i in range(3)]
    stg = [S.sb("stg%d" % i, [2, 512], F32) for i in range(2)]
    psb = [S.ps("pps%d" % i, [128, 512], F32) for i in range(2)]
    cnt = 0
    nld = 0
    for l in layers:
        S.dma("sp", g1.v, C.norm1_g.v[l:l + 1, :].pbc(2))
        S.dma("sp", g2.v, C.norm2_g.v[l:l + 1, :].pbc(2))
        if l % 2 == 1:
            S.dma("sp", psc.v, C.pool_scale.v[l // 2:l // 2 + 1, :].pbc(2))
        for seg in range(6):
            w = wb[nld % 3]
            br = brow[nld % 3]
            q = "sp" if nld % 2 == 0 else "act"
            nld += 1
            S.dma(q, w.v, C.ada_w.v[l].re("(k p) n -> p k n", p=128)[:, :, seg * D:(seg + 1) * D])
            S.dma(q, br.v, C.ada_b.v[l:l + 1, seg * D:(seg + 1) * D])
            for half in range(2):
                hs = slice(half * 512, (half + 1) * 512)
                ps = psb[cnt % 2]
                st = stg[cnt % 2]
                cnt += 1
                for k in range(8):
                    S.mm(ps[0:2, :], lhs2[:, k, :], w[:, k, hs], start=(k == 0), stop=False)
                S.mm(ps[0:2, :], C.onesf[0:1, 0:2], br[:, hs], start=False, stop=True)
                if seg == 1:
                    S.stt(st.v, ps[0:2, :], 1.0, g1[:, hs], ALU.add, ALU.mult)
                elif seg == 4:
                    S.stt(st.v, ps[0:2, :], 1.0, g2[:, hs], ALU.add, ALU.mult)
                elif seg == 2 and l % 2 == 1:
                    S.tt(st.v, ps[0:2, :], psc[:, hs], ALU.mult)
                else:
                    S.cp(st.v, ps[0:2, :])
                S.dma("pool", C.modtab.v[l, :, seg, hs], st.v)
    S.pop()


def load_mod(S, C, l, kind, seg, buf, q="sp"):
    S.dma(q, buf.v, C.modtab.v[l, kind, seg:seg + 1, :].pbc(128))


def moe_route(S, C, l, has_ctx, R):
    S.push()
    kinds = (0, 1) if has_ctx else (0,)
    A2 = {}
    B2 = {}
    for kd in kinds:
        A2[kd] = S.sb("A2_%d" % kd, [128, D], F32)
        B2[kd] = S.sb("B2_%d" % kd, [128, D], F32)
        load_mod(S, C, l, kd, 4, A2[kd])
        load_mod(S, C, l, kd, 3, B2[kd])
    rw = S.sb("rw", [128, 8, NE], F32)
    S.dma("sp", rw.v, C.router_w.v[l].re("(k p) e -> p k e", p=128))
    Ws = [norm_ws(S, "r%d" % i) for i in range(2)]
    xts = [S.sb("rxt%d" % i, [128, D], F32) for i in range(2)]
    h2f = [S.sb("h2f%d" % i, [128, D], F32) for i in range(2)]
    h2b = [S.sb("h2b%d" % i, [128, D], BF16) for i in range(2)]
    h2T = [S.sb("h2T%d" % i, [128, D], F32) for i in range(2)]
    ptr = [S.ps("rptr%d" % i, [128, D], F32) for i in range(2)]
    pl = [S.ps("rpl%d" % i, [128, 512], F32) for i in range(2)]
    mx = [S.sb("rmx%d" % i, [128, 1], F32) for i in range(2)]
    nmx = [S.sb("rnmx%d" % i, [128, 1], F32) for i in range(2)]
    sm = [S.sb("rsm%d" % i, [128, 1], F32) for i in range(2)]
    ex = [S.sb("rex%d" % i, [128, NE], F32) for i in range(2)]
    tiles = list(range(0 if has_ctx else 2, NT))

    def stage1(it):
        j = tiles[it]
        kd = 1 if j < 2 else 0
        i2 = it % 2
        xt = xts[i2]
        S.dma("sp", xt.v, C.xd.v[j * 128:(j + 1) * 128, :])
        hf = h2f[i2]
        hb = h2b[i2]
        norm_mod(S, C, xt.v, A2[kd].v, B2[kd].v, hf.v, Ws[i2])
        S.cp(hb.v, hf.v, eng="act")
        S.dma("pool", C.h2d.v[j * 128:(j + 1) * 128, :], hb.v)
        pt = ptr[i2]
        for k in range(8):
            S.tr(pt[:, k * 128:(k + 1) * 128], hf[:, k * 128:(k + 1) * 128], C.identf.v)

    def stage2(it):
        j = tiles[it]
        i2 = it % 2
        pt = ptr[i2]
        S.cp(h2T[i2][:, 0:512], pt[:, 0:512], eng="act")
        S.cp(h2T[i2][:, 512:1024], pt[:, 512:1024])
        for k in range(8):
            S.mm(pl[i2][:, 0:NE], h2T[i2][:, k * 128:(k + 1) * 128], rw[:, k, :], start=(k == 0), stop=(k == 7))
        S.red(mx[i2].v, pl[i2][:, 0:NE], ALU.max)
        S.ts(nmx[i2].v, mx[i2].v, -1.0, ALU.mult)
        S.act(ex[i2].v, pl[i2][:, 0:NE], AF.Exp, bias=nmx[i2].v, accum=sm[i2].v)
        S.recip(sm[i2].v, sm[i2].v)
        S.ts(R.aff[:, j, :], ex[i2].v, sm[i2].v, ALU.mult)

    stage1(0)
    for it in range(len(tiles)):
        if it + 1 < len(tiles):
            stage1(it + 1)
        stage2(it)
    S.pop()


def moe_bisect(S, C, R, specs):
    S.push()
    st = []
    for (j0, T, cap, pfx) in specs:
        o = NS()
        o.affv = R.aff[:, j0:j0 + T, :]
        o.T, o.cap = T, cap
        o.mid = S.sb(pfx + "mid", [128, NE], F32)
        o.dd = S.sb(pfx + "dd", [128, NE], F32)
        o.cntp = S.sb(pfx + "cntp", [128, NE], F32)
        o.cmp = S.sb(pfx + "cmp", [128, T, NE], F32)
        o.pc = S.ps(pfx + "pc", [128, 512], F32)
        o.lo = R.thr[pfx]
        S.memset(o.lo.v, 0.0)
        st.append(o)
    w = 0.5
    for it in range(27):
        for o in st:
            S.ts(o.mid.v, o.lo.v, w, ALU.add)
            S.tt(o.cmp.v, o.affv, o.mid.v.us(1).bc([128, o.T, NE]), ALU.is_ge)
            S.red(o.cntp.v, o.cmp.v.re("p t e -> p e t"), ALU.add)
            S.mm(o.pc[:, 0:NE], C.onesf.v, o.cntp.v)
            S.ts(o.dd.v, o.pc[:, 0:NE], float(o.cap), ALU.is_ge, w, ALU.mult)
            S.tt(o.lo.v, o.lo.v, o.dd.v, ALU.add)
        w *= 0.5
    S.pop()


def moe_topk(S, C, R, j0, T, cap, idx_out, gate_out, pfx):
    S.push()
    ncc = (cap + 127) // 128
    M = min(cap, 128)
    affv = R.aff[:, j0:j0 + T, :]
    lo = R.thr[pfx]
    mask = S.sb(pfx + "mask", [128, T, NE], F32)
    S.tt(mask.v, affv, lo.v.us(1).bc([128, T, NE]), ALU.is_ge)
    lstr = S.sb(pfx + "lstr", [128, 128], F32)
    S.dma("sp", lstr.v, C.h_lstrict.v)
    pw = S.ps(pfx + "pw", [128, 512], F32)
    ptot = S.ps(pfx + "ptot", [128, 512], F32)
    mflat = mask.v.re("p t e -> p (t e)")
    S.mm(pw[:, 0:T * NE], lstr.v, mflat)
    S.mm(ptot[:, 0:T * NE], C.onesf.v, mflat)
    totS = S.sb(pfx + "totS", [128, NE, T], F32)
    rsm = S.sb(pfx + "rsm", [128, NE, T], F32)
    offs = S.sb(pfx + "offs", [128, NE, T], F32)
    S.memset(totS.v, 0.0)
    S.memset(rsm.v, 1.0)
    S.memset(rsm[:, :, 0:1], 0.0)
    S.cp(totS[:, :, 1:T], ptot[:, 0:T * NE].re("p (t e) -> p e t", e=NE)[:, :, 0:T - 1])
    S.op("dve", lambda e: e.tensor_tensor_scan(offs.t[:].rearrange("p e t -> p (e t)"),
                                               rsm.t[:].rearrange("p e t -> p (e t)"),
                                               totS.t[:].rearrange("p e t -> p (e t)"),
                                               0.0, ALU.mult, ALU.add),
         reads=(rsm, totS), writes=(offs,))
    pos = S.sb(pfx + "pos", [128, T, NE], F32)
    S.tt(pos.v, pw[:, 0:T * NE].re("p (t e) -> p t e", e=NE), offs.v.re("p e t -> p t e"), ALU.add)
    BIG = 65536.0
    S.stt(pos.v, pos.v, -BIG, mask.v, ALU.add, ALU.mult)
    S.ts(pos.v, pos.v, BIG, ALU.add)
    tg = S.sb(pfx + "tg", [128, T, NE, 5], BF16)
    tka = S.sb(pfx + "tka", [128, NT], F32)
    tkb = S.sb(pfx + "tkb", [128, NT], F32)
    S.dma("sp", tka.v, C.h_toka.v)
    S.dma("sp", tkb.v, C.h_tokb.v)
    S.cp(tg[:, :, :, 0], tka[:, j0:j0 + T].us(2).bc([128, T, NE]))
    S.cp(tg[:, :, :, 1], tkb[:, j0:j0 + T].us(2).bc([128, T, NE]))
    r1 = S.sb(pfx + "r1", [128, T, NE], F32)
    r2 = S.sb(pfx + "r2", [128, T, NE], F32)
    S.cp(tg[:, :, :, 2], affv)
    S.tt(r1.v, affv, tg[:, :, :, 2], ALU.subtract)
    S.cp(tg[:, :, :, 3], r1.v)
    S.tt(r2.v, r1.v, tg[:, :, :, 3], ALU.subtract)
    S.cp(tg[:, :, :, 4], r2.v)
    iota32 = S.sb(pfx + "iota32", [128, 512], F32)
    S.dma("sp", iota32.v, C.h_iota.v)
    iota = S.sb(pfx + "iota", [128, 512], mybir.dt.int16)
    S.cp(iota.v, iota32.v)
    sel = [S.sb(pfx + "sel%d" % i, [128, 512], BF16) for i in range(4)]
    psI = [S.ps(pfx + "psI%d" % i, [128, 512], F32) for i in range(ncc)]
    t5 = [S.sb(pfx + "t5_%d" % i, [128, 5], F32) for i in range(4)]
    n = 0
    for e in range(NE):
        for jj in range(T):
            sb_ = sel[n % 4]
            n += 1
            S.ts(sb_[:, 0:cap], iota[:, 0:cap], pos[:, jj, e:e + 1], ALU.is_equal)
            for cc in range(ncc):
                S.mm(psI[cc][0:M, 0:5], sb_[:, cc * M:(cc + 1) * M], tg[:, jj, e, :],
                     start=(jj == 0), stop=(jj == T - 1))
        for cc in range(ncc):
            t_ = t5[cc]
            S.cp(t_[0:M, :], psI[cc][0:M, 0:5], eng="act")
            S.stt(idx_out[0:M, e, cc:cc + 1], t_[0:M, 0:1], 64.0, t_[0:M, 1:2], ALU.mult, ALU.add)
            S.red(gate_out[0:M, e, cc:cc + 1], t_[0:M, 2:5], ALU.add)
    S.pop()


def moe_experts(S, C, l, has_ctx, R):
    S.push()
    G5 = S.sb("G5", [128, D], F32)
    load_mod(S, C, l, 0, 5, G5)
    if has_ctx:
        G5c = S.sb("G5c", [128, D], F32)
        load_mod(S, C, l, 1, 5, G5c)
    NL = 512
    NC_ = 32 if has_ctx else 0
    NTOKE = NL + NC_
    wgb = [S.sb("wgb%d" % i, [128, 8, 512], BF16) for i in range(3)]
    wub = [S.sb("wub%d" % i, [128, 8, 512], BF16) for i in range(3)]
    wdb = [S.sb("wdb%d" % i, [128, 16, 512], BF16) for i in range(2)]
    xs = [S.sb("xs%d" % i, [128, 5, D], BF16) for i in range(2)]
    xsT = S.sb("xsT", [128, 8, 544], BF16)
    hidT = S.sb("hidT", [128, 16, 544], BF16)
    sg = [S.sb("sg%d" % i, [128, 544], F32) for i in range(2)]
    ost = [S.sb("ost%d" % i, [128, 5, D], F32) for i in range(2)]
    pts = [S.ps("ept%d" % i, [128, D], BF16) for i in range(2)]
    pb = [S.ps("epb%d" % i, [128, 512], F32) for i in range(6)]

    def load_gu(i):
        e, fb = divmod(i, 4)
        b = i % 3
        S.dma("pool", wgb[b].v, C.exp_wg.v[l, e].re("(k p) f -> p k f", p=128)[:, :, fb * 512:(fb + 1) * 512])
        S.dma("pool", wub[b].v, C.exp_wu.v[l, e].re("(k p) f -> p k f", p=128)[:, :, fb * 512:(fb + 1) * 512])

    def load_d(i):
        e, dh = divmod(i, 2)
        S.dma("pool", wdb[i % 2].v, C.exp_wd.v[l, e].re("(k p) d -> p k d", p=128)[:, :, dh * 512:(dh + 1) * 512])

    def gather(e):
        x_ = xs[e % 2]
        for cc in range(4):
            S.dma("pool", x_[:, cc, :], C.h2d.v, extra_reads=(R.idx.v,),
                  indirect=lambda en, cc=cc: en.indirect_dma_start(
                      out=x_.t[:, cc, :], out_offset=None, in_=C.h2d.t[:, :],
                      in_offset=bass.IndirectOffsetOnAxis(ap=R.idx.t[:, e, cc:cc + 1], axis=0)))
        if has_ctx:
            S.dma("pool", x_[0:32, 4, :], C.h2d.v, extra_reads=(R.idxc.v,),
                  indirect=lambda en: en.indirect_dma_start(
                      out=x_.t[0:32, 4, :], out_offset=None, in_=C.h2d.t[:, :],
                      in_offset=bass.IndirectOffsetOnAxis(ap=R.idxc.t[0:32, e, 0:1], axis=0)))

    gather(0)
    load_gu(0)
    load_gu(1)
    load_d(0)
    for e in range(NE):
        x_ = xs[e % 2]
        for cc in range(4):
            pt = pts[cc % 2]
            for k in range(8):
                S.tr(pt[:, k * 128:(k + 1) * 128], x_[:, cc, k * 128:(k + 1) * 128], C.identb.v)
            S.cp(xsT[:, :, cc * 128:(cc + 1) * 128], pt.v.re("p (k t) -> p k t", k=8), eng=("act" if cc % 2 else "dve"))
        if has_ctx:
            pt = pts[0]
            for k in range(8):
                S.tr(pt[:, k * 128:k * 128 + 32], x_[0:32, 4, k * 128:(k + 1) * 128], C.identb[0:32, 0:32])
            S.cp(xsT[:, :, 512:544], pt.v.re("p (k t) -> p k t", k=8)[:, :, 0:32])
        if e + 1 < NE:
            gather(e + 1)
        for fb in range(4):
            gi = e * 4 + fb
            if gi + 2 < NE * 4:
                load_gu(gi + 2)
            wg_, wu_ = wgb[gi % 3], wub[gi % 3]
            for fs in range(4):
                fc = fb * 4 + fs
                pg, pu = pb[(fc % 2) * 2], pb[(fc % 2) * 2 + 1]
                for k in range(8):
                    S.mm(pg.v, wg_[:, k, fs * 128:(fs + 1) * 128], xsT[:, k, 0:512], start=(k == 0), stop=(k == 7))
                for k in range(8):
                    S.mm(pu.v, wu_[:, k, fs * 128:(fs + 1) * 128], xsT[:, k, 0:512], start=(k == 0), stop=(k == 7))
                s_ = sg[fc % 2]
                S.act(s_[:, 0:512], pg.v, AF.Silu)
                S.tt(hidT[:, fc, 0:512], s_[:, 0:512], pu.v, ALU.mult)
                if has_ctx:
                    pgc, puc = pb[4], pb[5]
                    for k in range(8):
                        S.mm(pgc[:, 0:32], wg_[:, k, fs * 128:(fs + 1) * 128], xsT[:, k, 512:544], start=(k == 0), stop=(k == 7))
                    for k in range(8):
                        S.mm(puc[:, 0:32], wu_[:, k, fs * 128:(fs + 1) * 128], xsT[:, k, 512:544], start=(k == 0), stop=(k == 7))
                    S.act(s_[:, 512:544], pgc[:, 0:32], AF.Silu)
                    S.tt(hidT[:, fc, 512:544], s_[:, 512:544], puc[:, 0:32], ALU.mult)
        o_ = ost[e % 2]
        for dh in range(2):
            di = e * 2 + dh
            if di + 1 < NE * 2:
                load_d(di + 1)
            wd_ = wdb[di % 2]
            ds = slice(dh * 512, (dh + 1) * 512)
            for cc in range(4):
                po = pb[cc]
                for fk in range(16):
                    S.mm(po.v, hidT[:, fk, cc * 128:(cc + 1) * 128], wd_[:, fk, :], start=(fk == 0), stop=(fk == 15))
                S.stt(o_[:, cc, ds], po.v, R.gate[:, e, cc:cc + 1], G5[:, ds], ALU.mult, ALU.mult)
            if has_ctx:
                po = pb[4]
                for fk in range(16):
                    S.mm(po[0:32, :], hidT[:, fk, 512:544], wd_[:, fk, :], start=(fk == 0), stop=(fk == 15))
                S.stt(o_[0:32, 4, ds], po[0:32, :], R.gatec[0:32, e, 0:1], G5c[0:32, ds], ALU.mult, ALU.mult)
        for cc in range(4):
            S.dma("pool", C.xd.v, o_[:, cc, :], owner=o_, extra_reads=(R.idx.v,),
                  indirect=lambda en, cc=cc, o_=o_: en.indirect_dma_start(
                      out=C.xd.t[:, :], out_offset=bass.IndirectOffsetOnAxis(ap=R.idx.t[:, e, cc:cc + 1], axis=0),
                      in_=o_.t[:, cc, :], in_offset=None, compute_op=ALU.add))
        if has_ctx:
            S.dma("pool", C.xd.v, o_[0:32, 4, :], owner=o_, extra_reads=(R.idxc.v,),
                  indirect=lambda en, o_=o_: en.indirect_dma_start(
                      out=C.xd.t[:, :], out_offset=bass.IndirectOffsetOnAxis(ap=R.idxc.t[0:32, e, 0:1], axis=0),
                      in_=o_.t[0:32, 4, :], in_offset=None, compute_op=ALU.add))
    S.pop()


def moe_layer(S, C, l, has_ctx):
    S.push()
    R = NS()
    R.aff = S.sb("aff", [128, NT, NE], F32)
    R.idx = S.sb("idx", [128, NE, 4], I32)
    R.gate = S.sb("gate", [128, NE, 4], F32)
    R.idxc = S.sb("idxc", [128, NE, 1], I32)
    R.gatec = S.sb("gatec", [128, NE, 1], F32)
    R.thr = {"tl": S.sb("thr_l", [128, NE], F32), "tc": S.sb("thr_c", [128, NE], F32)}
    moe_route(S, C, l, has_ctx, R)
    moe_bisect(S, C, R, [(2, 32, 512, "tl")] + ([(0, 2, 32, "tc")] if has_ctx else []))
    moe_topk(S, C, R, 2, 32, 512, R.idx.v, R.gate.v, "tl")
    if has_ctx:
        moe_topk(S, C, R, 0, 2, 32, R.idxc.v, R.gatec.v, "tc")
    moe_experts(S, C, l, has_ctx, R)
    S.pop()


def odd_mixer(S, C, l, has_ctx):
    S.push()
    j = l // 2
    kinds = (0, 1) if has_ctx else (0,)
    A1, B1, G1 = {}, {}, {}
    for kd in kinds:
        A1[kd] = S.sb("A1_%d" % kd, [128, D], F32)
        B1[kd] = S.sb("B1_%d" % kd, [128, D], F32)
        G1[kd] = S.sb("G1_%d" % kd, [128, D], F32)
        load_mod(S, C, l, kd, 1, A1[kd])
        load_mod(S, C, l, kd, 0, B1[kd])
        load_mod(S, C, l, kd, 2, G1[kd])
    band = S.sb("band", [128, 4, 5, 128], F32)
    S.dma("sp", band.v.re("p g v t -> p (g v t)"), C.h_band.v)
    wp = S.sb("wp", [128, 8, 256], BF16)
    S.dma("pool", wp.v, C.pool_w.v[j].re("g (h p) d -> p (g h) d", p=128))
    Ws = [norm_ws(S, "o%d" % i) for i in range(2)]
    xts = [S.sb("oxt%d" % i, [128, D], F32) for i in range(4)]
    hs = [S.sb("oh%d" % i, [128, D], F32) for i in range(4)]
    ppT = S.sb("ppT", [128, 8, 128], BF16)
    yt = S.sb("oyt", [128, D], F32)
    xn = [S.sb("oxn%d" % i, [128, D], F32) for i in range(2)]
    pp = S.ps("opp", [128, D], F32)
    py = S.ps("opy", [128, D], F32)

    def prep(t):
        kd = 1 if t < 2 else 0
        S.dma("sp", xts[t % 4].v, C.xd.v[t * 128:(t + 1) * 128, :])
        norm_mod(S, C, xts[t % 4].v, A1[kd].v, B1[kd].v, hs[t % 4].v, Ws[t % 2])

    seqs = ([(0, 2)] if has_ctx else []) + [(2, NT)]
    for (t0, t1) in seqs:
        prep(t0)
        for t in range(t0, t1):
            if t + 1 < t1:
                prep(t + 1)
            kd = 1 if t < 2 else 0
            first, last = (t == t0), (t == t1 - 1)
            for ci in range(8):
                g = ci // 2
                terms = []
                if not first:
                    terms.append((t - 1, 0))
                terms.append((t, 3 if first else (4 if last else 1)))
                if not last:
                    terms.append((t + 1, 2))
                for n_, (tj, var) in enumerate(terms):
                    S.mm(pp[:, ci * 128:(ci + 1) * 128], hs[tj % 4][:, ci * 128:(ci + 1) * 128], band[:, g, var, :],
                         start=(n_ == 0), stop=(n_ == len(terms) - 1))
            S.cp(ppT.v.re("p c t -> p (c t)"), pp.v, eng="act")
            for g in range(4):
                for hf in range(2):
                    S.mm(py[:, g * 256:(g + 1) * 256], ppT[:, g * 2 + hf, :], wp[:, g * 2 + hf, :],
                         start=(hf == 0), stop=(hf == 1))
            S.tt(yt.v, py.v, G1[kd].v, ALU.mult)
            xo = xn[t % 2]
            S.tt(xo.v, xts[t % 4].v, yt.v, ALU.add)
            S.dma("pool", C.xd.v[t * 128:(t + 1) * 128, :], xo.v)
    S.pop()


def final_norm(S, C):
    S.push()
    gf = S.sb("gf", [128, D], F32)
    S.dma("sp", gf.v, C.final_g.v.pbc(128))
    Ws = [norm_ws(S, "f%d" % i) for i in range(2)]
    xts = [S.sb("fxt%d" % i, [128, D], F32) for i in range(2)]
    ots = [S.sb("fot%d" % i, [128, D], F32) for i in range(2)]
    for t in range(2, NT):
        xt = xts[t % 2]
        S.dma("sp", xt.v, C.xd.v[t * 128:(t + 1) * 128, :])
        W = Ws[t % 2]
        S.act(W.junk.v, xt.v, AF.Square, accum=W.ssq.v)
        S.act(W.lnv.v, W.ssq.v, AF.Ln, scale=1.0 / D, bias=C.epsb.v)
        S.act(W.rstd.v, W.lnv.v, AF.Exp, scale=-0.5)
        S.stt(ots[t % 2].v, xt.v, W.rstd.v, gf.v, ALU.mult, ALU.mult)
        S.dma("pool", C.out.v[(t - 2) * 128:(t - 1) * 128, :], ots[t % 2].v)
    S.pop()


BLOCKS = [(0, 256)] + [(256 + 512 * i, 512) for i in range(8)]


def even_E1(S, C, l, hT):
    S.push()
    A1, B1 = {}, {}
    for kd in (0, 1):
        A1[kd] = S.sb("eA1_%d" % kd, [128, D], F32)
        B1[kd] = S.sb("eB1_%d" % kd, [128, D], F32)
        load_mod(S, C, l, kd, 1, A1[kd])
        load_mod(S, C, l, kd, 0, B1[kd])
    Ws = [norm_ws(S, "e%d" % i) for i in range(2)]
    xts = [S.sb("ext%d" % i, [128, D], F32) for i in range(2)]
    hb = [S.sb("ehb%d" % i, [128, D], BF16) for i in range(2)]
    pt = [S.ps("ept%d" % i, [128, D], BF16) for i in range(2)]
    def stage1(t):
        kd = 1 if t < 2 else 0
        xt = xts[t % 2]
        S.dma("sp", xt.v, C.xd.v[t * 128:(t + 1) * 128, :])
        norm_mod(S, C, xt.v, A1[kd].v, B1[kd].v, hb[t % 2].v, Ws[t % 2])

    def stage2(t):
        for k in range(8):
            S.tr(pt[t % 2][:, k * 128:(k + 1) * 128], hb[t % 2][:, k * 128:(k + 1) * 128], C.identb.v)
        S.cp(hT[:, :, t * 128:(t + 1) * 128], pt[t % 2].v.re("p (k t) -> p k t", k=8), eng="act")

    stage1(0)
    for t in range(NT):
        if t + 1 < NT:
            stage1(t + 1)
        stage2(t)
    S.pop()


def hgrn_proj(S, C, l, hT, HG):
    j = l // 2
    S.push()
    rmask = S.sb("rmask", [128, 512], F32)
    S.dma("sp", rmask.v, C.h_rmask.v)
    lbin = S.sb("lbin", [128, 16], F32)
    S.dma("sp", lbin.v, C.hg_lb_fm.v)
    lbv = S.sb("lbv", [128, 8], F32)
    oml = S.sb("oml", [128, 8], F32)
    if j == 0:
        S.memset(lbv.v, 0.0)
    else:
        S.tt(lbv.v, lbin[:, 8:16], lbin[:, 0:8], ALU.subtract)
        S.act(lbv.v, lbv.v, AF.Sigmoid)
    S.ts(oml.v, lbv.v, -1.0, ALU.mult, 1.0, ALU.add)
    wall = {}
    for nm, off in (("q", O_Q), ("zf", O_FFW), ("zb", O_FBW), ("i", O_I), ("g", O_G)):
        wall[nm] = S.sb("wa_" + nm, [128, 8, 512], BF16)
        S.dma("pool", wall[nm].v, C.w_in.v[j].re("(k p) c -> p k c", p=128)[:, :, off:off + 512])
    wks = [{nm: S.sb("hw%d_" % i + nm, [128, 512], F32) for nm in ("q32", "s32", "lf", "k32", "cb", "d1", "d2", "eq", "ek", "ef", "eh", "ez", "den")}
           for i in range(2)]
    qkb = [[S.sb("qkb%d_%d" % (d, i), [128, 2, 512], BF16) for i in range(2)] for d in range(2)]
    khb = [S.sb("khb%d" % i, [128, 512], BF16) for i in range(2)]
    khT = [S.sb("khTs%d" % i, [128, 4, 128], BF16) for i in range(2)]
    vst = [S.sb("vst%d" % i, [128, 512], BF16) for i in range(2)]
    gst = [S.sb("gst%d" % i, [128, 512], BF16) for i in range(2)]
    pj = [S.ps("hpj%d" % i, [128, 512], F32) for i in range(3)]
    pvg = [S.ps("hpvg%d" % i, [128, 512], F32) for i in range(2)]
    ptb = [S.ps("hptb%d" % i, [128, 512], BF16) for i in range(2)]
    for t in range(NT):
        for k in range(8):
            S.mm(pvg[0].v, hT[:, k, t * 128:(t + 1) * 128], wall["i"][:, k, :], start=(k == 0), stop=(k == 7))
        for k in range(8):
            S.mm(pvg[1].v, hT[:, k, t * 128:(t + 1) * 128], wall["g"][:, k, :], start=(k == 0), stop=(k == 7))
        S.cp(vst[t % 2].v, pvg[0].v)
        S.act(gst[t % 2].v, pvg[1].v, AF.Silu)
        S.dma("sp", C.vh_d.v[:, t, :], vst[t % 2].v)
        S.dma("sp", C.sg_d.v[:, t, :], gst[t % 2].v)
    n_it = 0
    for h in range(4):
        hs = slice(h * 128, (h + 1) * 128)
        for (t0, n) in BLOCKS:
            nch = n // 64
            c0 = t0 // 64
            ts_ = slice(t0, t0 + n)
            pq = pj[0]
            for k in range(8):
                S.mm(pq[:, 0:n], wall["q"][:, k, hs], hT[:, k, ts_], start=(k == 0), stop=(k == 7))
            q32 = wks[n_it % 2]["q32"]
            S.cp(q32[:, 0:n], pq[:, 0:n], eng="act")
            mcols = (31, 32)
            lasts = (63, 0)
            for d in range(2):
                wk = wks[d]
                wz = wall["zf"] if d == 0 else wall["zb"]
                pz = pj[1 + d]
                for k in range(8):
                    S.mm(pz[:, 0:n], wz[:, k, hs], hT[:, k, ts_], start=(k == 0), stop=(k == 7))
                S.act(wk["ez"][:, 0:n], pz[:, 0:n], AF.Exp, scale=-1.0)
                S.ts(wk["den"][:, 0:n], wk["ez"][:, 0:n], 1.0, ALU.add)
                S.recip(wk["s32"][:, 0:n], wk["den"][:, 0:n])
                if j != 0:
                    S.ts(wk["s32"][:, 0:n], wk["s32"][:, 0:n], oml[:, d * 4 + h:d * 4 + h + 1], ALU.mult,
                         lbv[:, d * 4 + h:d * 4 + h + 1], ALU.add)
            for d in range(2):
                wk = wks[d]
                S.act(wk["lf"][:, 0:n], wk["s32"][:, 0:n], AF.Ln)
                S.ts(wk["k32"][:, 0:n], wk["s32"][:, 0:n], -1.0, ALU.mult, 1.0, ALU.add, eng="pool")
            for d in range(2):
                wk = wks[d]
                lf, cb, d1, d2 = wk["lf"], wk["cb"], wk["d1"], wk["d2"]
                S.op("dve", lambda e, n=n, cb=cb, lf=lf: e.tensor_tensor_scan(cb.t[:, 0:n], rmask.t[:, 0:n], lf.t[:, 0:n],
                                                                              0.0, ALU.mult, ALU.add),
                     reads=(rmask, lf), writes=(cb,))
                cb3 = cb[:, 0:n].re("p (c s) -> p c s", s=64)
                d13 = d1[:, 0:n].re("p (c s) -> p c s", s=64)
                d23 = d2[:, 0:n].re("p (c s) -> p c s", s=64)
                if d == 1:
                    S.tt(d13, cb3[:, :, 63:64].bc([128, nch, 64]), cb3, ALU.subtract)
                    S.tt(cb[:, 0:n], d1[:, 0:n], lf[:, 0:n], ALU.add)
                mcol, last = mcols[d], lasts[d]
                S.tt(d13, cb3, cb3[:, :, mcol:mcol + 1].bc([128, nch, 64]), ALU.subtract)
                S.tt(d23, cb3, cb3[:, :, last:last + 1].bc([128, nch, 64]), ALU.subtract)
            for d in range(2):
                wk = wks[d]
                S.act(wk["eq"][:, 0:n], wk["d1"][:, 0:n], AF.Exp)
                S.act(wk["ek"][:, 0:n], wk["d1"][:, 0:n], AF.Exp, scale=-1.0)
                S.act(wk["ef"][:, 0:n], wk["cb"][:, 0:n], AF.Exp)
                S.act(wk["eh"][:, 0:n], wk["d2"][:, 0:n], AF.Exp, scale=-1.0)
            for d in range(2):
                wk = wks[d]
                k32, eq, ek, ef, eh = wk["k32"], wk["eq"], wk["ek"], wk["ef"], wk["eh"]
                mcol, last = mcols[d], lasts[d]
                qb = qkb[d][n_it % 2]
                S.tt(qb[:, 0, 0:n], q32[:, 0:n], eq[:, 0:n], ALU.mult)
                S.tt(qb[:, 1, 0:n], k32[:, 0:n], ek[:, 0:n], ALU.mult, eng="pool")
                kh_ = khb[d]
                S.tt(kh_[:, 0:n], k32[:, 0:n], eh[:, 0:n], ALU.mult, eng="pool")
                ef3 = ef[:, 0:n].re("p (c s) -> p c s", s=64)
                S.cp(HG.dec[:, d, h, c0:c0 + nch], ef3[:, :, last])
                S.cp(HG.expm[:, d, h, c0:c0 + nch], ef3[:, :, mcol])
                S.dma("sp", C.qk_d.v[d][:, h, :, ts_], qb[:, :, 0:n])
                pt_ = ptb[d]
                kt_s = khT[d]
                ntl = n // 128
                for i in range(ntl):
                    S.tr(pt_[:, i * 128:(i + 1) * 128], kh_[:, i * 128:(i + 1) * 128], C.identb.v)
                S.cp(kt_s[:, 0:ntl, :], pt_[:, 0:n].re("p (t k) -> p t k", k=128), eng="act")
                S.dma("sp", C.khT_d.v[d][:, t0 // 128:t0 // 128 + ntl, h, :], kt_s[:, 0:ntl, :])
            n_it += 1
    S.pop()


def hgrn_scan(S, C, l, ctx_out, HG):
    j = l // 2
    S.push()
    v_all = S.sb("v_all", [128, NT, 512], BF16)
    S.dma("sp", v_all.v, C.vh_d.v)
    oacc = S.sb("oacc", [128, NT, 512], F32)
    S.memset(oacc.v, 0.0, eng="pool")
    trimf = S.sb("trimf", [128, 2, 64], F32)
    S.dma("sp", trimf.v, C.h_trimask.v)
    maskI = S.sb("maskI", [128, 2, 4, 64], I32)
    for h in range(4):
        S.cp(maskI[:, :, h, :], trimf.v)
    hgg = S.sb("hgg", [128, 128], F32)
    S.dma("sp", hgg.v, C.hg_norm_g.v[j:j + 1, :].pbc(128))
    Sf = [[S.sb("Sf%d_%d" % (d, h), [128, 128], F32) for h in range(4)] for d in range(2)]
    Sb = [[S.sb("Sb%d_%d" % (d, h), [128, 128], BF16) for h in range(4)] for d in range(2)]
    attT = [S.sb("attT%d" % d, [128, 4, 64], BF16) for d in range(2)]
    qkblk = [[S.sb("qkblk%d_%d" % (d, i), [128, 4, 2, 512], BF16) for i in range(2)] for d in range(2)]
    khblk = [[S.sb("khblk%d_%d" % (d, i), [128, 4, 4, 128], BF16) for i in range(2)] for d in range(2)]
    psA = [S.ps("hpsA%d" % d, [128, 512], F32) for d in range(2)]
    psO = [S.ps("hpsO%d" % d, [128, 512], F32) for d in range(2)]
    psU = [S.ps("hpsU%d" % d, [128, 512], F32) for d in range(2)]
    for d in range(2):
        for h in range(4):
            S.memset(Sf[d][h].v, 0.0)
            S.memset(Sb[d][h].v, 0.0)
        S.memset(attT[d].v, 0.0)
    bseq = [list(range(9)), [0] + list(range(8, 0, -1))]
    order = [list(range(68)), [3, 2, 1, 0] + list(range(67, 3, -1))]
    dq = ["sp", "pool"]

    def load_block(d, si):
        b = bseq[d][si]
        t0, n = BLOCKS[b]
        S.dma(dq[d], qkblk[d][si % 2][:, :, :, 0:n], C.qk_d.v[d][:, :, :, t0:t0 + n])
        S.dma(dq[d], khblk[d][si % 2][:, 0:n // 128, :, :], C.khT_d.v[d][:, t0 // 128:t0 // 128 + n // 128, :, :])

    cur_si = [-1, -1]
    for d in range(2):
        load_block(d, 0)
        load_block(d, 1)
    for step in range(68):
        inf = []
        for d in range(2):
            c = order[d][step]
            b = 0 if c < 4 else 1 + (c - 4) // 8
            si = bseq[d].index(b)
            if si != cur_si[d]:
                cur_si[d] = si
                if si >= 1 and si + 1 < 9:
                    load_block(d, si + 1)
            t0b, nb = BLOCKS[b]
            lc = c - t0b // 64
            o = NS()
            o.c = c
            o.ls = slice(lc * 64, lc * 64 + 64)
            o.tl = (c // 2) - t0b // 128
            o.t = c // 2
            pb_ = (c % 2) * 64
            o.rows = slice(pb_, pb_ + 64)
            o.qk = qkblk[d][si % 2]
            o.kh = khblk[d][si % 2]
            o.out = ctx_out or c >= 4
            inf.append(o)
        for d in range(2):
            o = inf[d]
            if o.out:
                for h in range(4):
                    S.mm(psA[d][o.rows, h * 64:(h + 1) * 64], o.qk[:, h, 1, o.ls], o.qk[:, h, 0, o.ls])
                S.op("dve", lambda e, d=d, rows=o.rows: e.copy_predicated(
                    attT[d].t[rows, :, :], maskI.t[rows, d, :, :],
                    psA[d].t[rows, 0:256].rearrange("p (h t) -> p h t", h=4)),
                    reads=(maskI, psA[d]), writes=(attT[d],))
        if step < 67:
            for d in range(2):
                o = inf[d]
                for h in range(4):
                    hs = slice(h * 128, (h + 1) * 128)
                    S.mm(psU[d][:, hs], o.kh[o.rows, o.tl, h, :], v_all[o.rows, o.t, hs])
        for d in range(2):
            o = inf[d]
            if o.out:
                for h in range(4):
                    hs = slice(h * 128, (h + 1) * 128)
                    S.mm(psO[d][o.rows, hs], attT[d][o.rows, h, :], v_all[o.rows, o.t, hs], start=True, stop=False)
                    S.mm(psO[d][o.rows, hs], o.qk[:, h, 0, o.ls], Sb[d][h].v, start=False, stop=True)
                S.tt(oacc[o.rows, o.t, :], oacc[o.rows, o.t, :], psO[d][o.rows, :], ALU.add)
        if step < 67:
            for d in range(2):
                o = inf[d]
                cn = order[d][step + 1]
                for h in range(4):
                    hs = slice(h * 128, (h + 1) * 128)
                    S.stt(Sf[d][h].v, Sf[d][h].v, HG.dec[:, d, h, o.c:o.c + 1], psU[d][:, hs], ALU.mult, ALU.add)
                    S.act(Sb[d][h].v, Sf[d][h].v, AF.Identity, scale=HG.expm[:, d, h, cn:cn + 1])
    tmin = 0 if ctx_out else 2
    sq = S.sb("hsq", [128, 4, 512], F32)
    ss = S.sb("hss", [128, 16], F32)
    sgb = [S.sb("hsgb%d" % i, [128, 4, 512], BF16) for i in range(2)]
    hgo = [S.sb("hgo%d" % i, [128, 4, 512], BF16) for i in range(2)]
    mv = C.mixd.v.re("(t p) f -> p t f", p=128)
    for gi, t4 in enumerate(range(tmin, NT, 4)):
        n4 = min(4, NT - t4)
        o4 = oacc[:, t4:t4 + n4, :]
        S.dma("sp", sgb[gi % 2][:, 0:n4, :], C.sg_d.v[:, t4:t4 + n4, :])
        S.tt(sq[:, 0:n4, :], o4, o4, ALU.mult)
        S.red(ss[:, 0:n4 * 4], sq[:, 0:n4, :].re("p t (h v) -> p (t h) v", h=4), ALU.add)
        S.act(ss[:, 0:n4 * 4], ss[:, 0:n4 * 4], AF.Ln, scale=1.0 / 128, bias=C.epsb.v)
        S.act(ss[:, 0:n4 * 4], ss[:, 0:n4 * 4], AF.Exp, scale=-0.5)
        o4h = o4.re("p t (h v) -> p (t h) v", h=4)
        S.tt(o4h, o4h, ss[:, 0:n4 * 4].us(2).bc([128, n4 * 4, 128]), ALU.mult)
        S.tt(o4h, o4h, hgg.v.us(1).bc([128, n4 * 4, 128]), ALU.mult)
        S.tt(hgo[gi % 2][:, 0:n4, :], o4, sgb[gi % 2][:, 0:n4, :], ALU.mult)
        S.dma("pool", mv[:, t4:t4 + n4, 0:512], hgo[gi % 2][:, 0:n4, :])
    S.pop()


def mla_prep(S, C, l, hT):
    j = l // 2
    S.push()
    wm = S.sb("wm", [128, 8, 448], BF16)
    S.dma("pool", wm.v, C.w_in.v[j].re("(k p) c -> p k c", p=128)[:, :, O_QA:3008])
    wmrot = S.sb("wmrot", [128, 8, 64], BF16)
    pe5 = wm[:, :, 384:448].re("p k (a two s) -> p k a two s", a=2, two=2)
    ro5 = wmrot.v.re("p k (a two s) -> p k a two s", a=2, two=2)
    S.ts(ro5[:, :, :, 0, :], pe5[:, :, :, 1, :], -1.0, ALU.mult)
    S.cp(ro5[:, :, :, 1, :], pe5[:, :, :, 0, :])
    wkvb = S.sb("wkvb", [128, 1024], BF16)
    S.dma("pool", wkvb.v, C.mla_wkv_b.v[j])
    kvg = S.sb("kvg", [128, 2], F32)
    S.dma("sp", kvg.v, C.mla_kvn_g_fm.v)
    qng = S.sb("qng", [128, 4], F32)
    S.dma("sp", qng.v, C.mla_qn_g_fm.v)
    cosb = [S.sb("pcos%d" % i, [64, 512], F32) for i in range(2)]
    sinb = [S.sb("psin%d" % i, [64, 512], F32) for i in range(2)]
    sq = [S.sb("psq%d" % i, [128, 512], F32) for i in range(2)]
    rstd = S.sb("prstd", [128, 512], F32)
    kvn = S.sb("kvn", [128, 512], BF16)
    knb = [S.sb("knb%d" % i, [128, 4, 512], BF16) for i in range(2)]
    vb = [S.sb("vb%d" % i, [128, 4, 4, 130], BF16) for i in range(2)]
    for i in range(2):
        S.memset(vb[i].v, 0.0)
        S.memset(vb[i][:, :, :, 128:129], 1.0)
    t1 = S.sb("pt1", [64, 512], F32)
    t2 = S.sb("pt2", [64, 512], F32)
    kpb = [S.sb("kpb%d" % i, [64, 512], BF16) for i in range(2)]
    qnb = [S.sb("qnb%d" % i, [128, 2, 512], BF16) for i in range(2)]
    P = [S.ps("mpP%d" % i, [128, 512], F32) for i in range(7)]
    wv = wkvb.v.re("r (h two c) -> r h two c", h=4, two=2)[:, :, 1, :]
    for bi, (t0, n) in enumerate(BLOCKS):
        ts_ = slice(t0, t0 + n)
        b2 = bi % 2
        S.dma("sp", cosb[b2][:, 0:n], C.h_cosT.v[:, ts_])
        S.dma("sp", sinb[b2][:, 0:n], C.h_sinT.v[:, ts_])
        for k in range(8):
            S.mm(P[0][:, 0:n], wm[:, k, 256:384], hT[:, k, ts_], start=(k == 0), stop=(k == 7))
        S.act(sq[0][:, 0:n], P[0][:, 0:n], AF.Square)
        S.mm(P[1][:, 0:n], C.onesf.v, sq[0][:, 0:n])
        S.act(rstd[:, 0:n], P[1][:, 0:n], AF.Ln, scale=1.0 / 128, bias=C.epsb.v)
        S.act(rstd[:, 0:n], rstd[:, 0:n], AF.Exp, scale=-0.5)
        S.stt(kvn[:, 0:n], P[0][:, 0:n], kvg[:, j:j + 1], rstd[:, 0:n], ALU.mult, ALU.mult)
        for h in range(4):
            pk = P[2 + h % 2]
            S.mm(pk[:, 0:n], wkvb[:, h * 256:h * 256 + 128], kvn[:, 0:n])
            S.cp(knb[b2][:, h, 0:n], pk[:, 0:n], eng=("act" if h % 2 else "dve"))
        S.dma("sp", C.knT_d.v[:, :, ts_], knb[b2][:, :, 0:n])
        for sub in range(n // 128):
            S.mm(P[4].v.re("p (h c) -> p h c", h=4), kvn[:, sub * 128:(sub + 1) * 128], wv)
            S.cp(vb[b2][:, sub, :, 0:128], P[4].v.re("p (h c) -> p h c", h=4))
        S.dma("sp", C.v_d.v[:, t0 // 128:t0 // 128 + n // 128, :, :], vb[b2][:, 0:n // 128, :, :])
        for k in range(8):
            S.mm(P[5][0:64, 0:n], wm[:, k, 384:448], hT[:, k, ts_], start=(k == 0), stop=(k == 7))
        for k in range(8):
            S.mm(P[6][0:64, 0:n], wmrot[:, k, :], hT[:, k, ts_], start=(k == 0), stop=(k == 7))
        S.tt(t1[:, 0:n], P[5][0:64, 0:n], cosb[b2][:, 0:n], ALU.mult)
        S.tt(t2[:, 0:n], P[6][0:64, 0:n], sinb[b2][:, 0:n], ALU.mult)
        S.tt(kpb[b2][:, 0:n], t1[:, 0:n], t2[:, 0:n], ALU.add)
        S.dma("sp", C.kpeT_d.v[:, ts_], kpb[b2][:, 0:n])
        pq = [P[0], P[2]]
        for c2 in range(2):
            for k in range(8):
                S.mm(pq[c2][:, 0:n], wm[:, k, c2 * 128:(c2 + 1) * 128], hT[:, k, ts_], start=(k == 0), stop=(k == 7))
            S.act(sq[c2][:, 0:n], pq[c2][:, 0:n], AF.Square)
        for c2 in range(2):
            S.mm(P[1][:, 0:n], C.onesf.v, sq[c2][:, 0:n], start=(c2 == 0), stop=(c2 == 1))
        S.act(rstd[:, 0:n], P[1][:, 0:n], AF.Ln, scale=1.0 / 256, bias=C.epsb.v)
        S.act(rstd[:, 0:n], rstd[:, 0:n], AF.Exp, scale=-0.5)
        for c2 in range(2):
            S.stt(qnb[b2][:, c2, 0:n], pq[c2][:, 0:n], qng[:, j * 2 + c2:j * 2 + c2 + 1], rstd[:, 0:n], ALU.mult, ALU.mult)
        S.dma("sp", C.qnT_d.v[:, :, ts_], qnb[b2][:, :, 0:n])
    S.pop()


def attention(S, C, l, ctx_out):
    j = l // 2
    S.push()
    knT = S.sb("knT", [128, 4, NTOK], BF16)
    kpeT = S.sb("kpeT", [64, NTOK], BF16)
    vall = S.sb("vall", [128, NT, 4, 130], BF16)
    qnT = S.sb("qnT", [128, 2, NTOK], BF16)
    S.dma("sp", knT.v, C.knT_d.v)
    S.dma("sp", kpeT.v, C.kpeT_d.v)
    S.dma("sp", vall.v, C.v_d.v)
    S.dma("sp", qnT.v, C.qnT_d.v)
    wqb = S.sb("wqb", [128, 2, 768], BF16)
    S.dma("pool", wqb.v, C.mla_wq_b.v[j].re("(k p) c -> p k c", p=128))
    wqrot = S.sb("wqrot", [128, 2, 4, 64], BF16)
    for kc in range(2):
        src = wqb[:, kc, :].re("p (h c) -> p h c", h=4)[:, :, 128:192].re("p h (a two s) -> p h a two s", a=2, two=2)
        dst = wqrot[:, kc, :, :].re("p h (a two s) -> p h a two s", a=2, two=2)
        for a in range(2):
            S.ts(dst[:, :, a, 0, :], src[:, :, a, 1, :], -1.0, ALU.mult)
            S.cp(dst[:, :, a, 1, :], src[:, :, a, 0, :])
    cosb = [S.sb("acos%d" % i, [64, 512], F32) for i in range(2)]
    sinb = [S.sb("asin%d" % i, [64, 512], F32) for i in range(2)]
    qhn = [S.sb("qhn%d" % i, [128, 512], BF16) for i in range(2)]
    qhp = [S.sb("qhp%d" % i, [64, 512], BF16) for i in range(2)]
    t1 = S.sb("at1", [64, 512], F32)
    t2 = S.sb("at2", [64, 512], F32)
    PT = [S.sb("aPT%d" % i, [128, 512], BF16) for i in range(2)]
    mo = [S.sb("amo%d" % i, [128, 4, 512], BF16) for i in range(2)]
    rec = S.sb("arec", [128, 4], F32)
    psS = [S.ps("apsS%d" % i, [128, 512], F32) for i in range(2)]
    psO = [S.ps("apsO%d" % i, [128, 512], F32) for i in range(4)]
    pqa = S.ps("apqa", [128, 512], F32)
    pqb = S.ps("apqb", [128, 512], F32)
    qblocks = ([(0, 256, 2)] if ctx_out else []) + [(256 + 512 * i, 512, NT) for i in range(8)]
    mv = C.mixd.v.re("(t p) f -> p t f", p=128)
    cnt = 0
    for bi, (t0, n, nk) in enumerate(qblocks):
        nq = n // 128
        ts_ = slice(t0, t0 + n)
        b2 = bi % 2
        S.dma("sp", cosb[b2][:, 0:n], C.h_cosT.v[:, ts_])
        S.dma("sp", sinb[b2][:, 0:n], C.h_sinT.v[:, ts_])
        for h in range(4):
            hh = (bi * 4 + h) % 2
            for kc in range(2):
                S.mm(pqa[:, 0:n], wqb[:, kc, h * 192:h * 192 + 128], qnT[:, kc, ts_], start=(kc == 0), stop=(kc == 1))
            S.cp(qhn[hh][:, 0:n], pqa[:, 0:n], eng="act")
            for kc in range(2):
                S.mm(pqb[0:64, 0:n], wqb[:, kc, h * 192 + 128:h * 192 + 192], qnT[:, kc, ts_], start=(kc == 0), stop=(kc == 1))
            S.tt(t1[:, 0:n], pqb[0:64, 0:n], cosb[b2][:, 0:n], ALU.mult)
            for kc in range(2):
                S.mm(pqa[0:64, 0:n], wqrot[:, kc, h, :], qnT[:, kc, ts_], start=(kc == 0), stop=(kc == 1))
            S.tt(t2[:, 0:n], pqa[0:64, 0:n], sinb[b2][:, 0:n], ALU.mult)
            S.tt(qhp[hh][:, 0:n], t1[:, 0:n], t2[:, 0:n], ALU.add)
            def emit_pv(kt_, pt_):
                for qs in range(nq):
                    S.mm(psO[qs][:, 0:129], pt_[:, qs * 128:(qs + 1) * 128], vall[:, kt_, h, 0:129],
                         start=(kt_ == 0), stop=(kt_ == nk - 1))
            pend = None
            for kt_ in range(nk):
                ps_ = psS[cnt % 2]
                pt_ = PT[cnt % 2]
                cnt += 1
                ks = slice(kt_ * 128, (kt_ + 1) * 128)
                S.mm(ps_[:, 0:n], knT[:, h, ks], qhn[hh][:, 0:n], start=True, stop=False)
                S.mm(ps_[:, 0:n], kpeT[:, ks], qhp[hh][:, 0:n], start=False, stop=True)
                if pend is not None:
                    emit_pv(*pend)
                S.act(pt_[:, 0:n], ps_[:, 0:n], AF.Exp, scale=MLA_SCALE)
                pend = (kt_, pt_)
            emit_pv(*pend)
            for qs in range(nq):
                S.recip(rec[:, qs:qs + 1], psO[qs][:, 128:129])
                S.ts(mo[b2][:, qs, h * 128:(h + 1) * 128], psO[qs][:, 0:128], rec[:, qs:qs + 1], ALU.mult)
        S.dma("sp", mv[:, t0 // 128:t0 // 128 + nq, 512:1024], mo[b2][:, 0:nq, :])
    S.pop()


def out_proj(S, C, l, ctx_out):
    j = l // 2
    S.push()
    wo = S.sb("wo", [128, 8, D], BF16)
    S.dma("pool", wo.v, C.w_out.v[j].re("(k p) d -> p k d", p=128))
    kinds = (0, 1) if ctx_out else (0,)
    G1 = {}
    for kd in kinds:
        G1[kd] = S.sb("pG1_%d" % kd, [128, D], F32)
        load_mod(S, C, l, kd, 2, G1[kd])
    mt = [S.sb("pmt%d" % i, [128, D], BF16) for i in range(2)]
    xts = [S.sb("pxt%d" % i, [128, D], F32) for i in range(2)]
    mT = [S.sb("pmT%d" % i, [128, 8, 128], BF16) for i in range(2)]
    yt = S.sb("pyt", [128, D], F32)
    xn = [S.sb("pxn%d" % i, [128, D], F32) for i in range(2)]
    pt = [S.ps("ppt%d" % i, [128, D], BF16) for i in range(2)]
    py = [S.ps("ppy%d" % i, [128, D], F32) for i in range(2)]
    tl_ = list(range(0 if ctx_out else 2, NT))

    def stage1(it):
        t = tl_[it]
        i2 = it % 2
        S.dma("sp", mt[i2].v, C.mixd.v[t * 128:(t + 1) * 128, :])
        S.dma("sp", xts[i2].v, C.xd.v[t * 128:(t + 1) * 128, :])
        for k in range(8):
            S.tr(pt[i2][:, k * 128:(k + 1) * 128], mt[i2][:, k * 128:(k + 1) * 128], C.identb.v)

    def stage2(it):
        t = tl_[it]
        kd = 1 if t < 2 else 0
        i2 = it % 2
        S.cp(mT[i2].v.re("p k t -> p (k t)"), pt[i2].v, eng="act")
        for hf in range(2):
            for k in range(8):
                S.mm(py[i2][:, hf * 512:(hf + 1) * 512], mT[i2][:, k, :], wo[:, k, hf * 512:(hf + 1) * 512],
                     start=(k == 0), stop=(k == 7))
        S.tt(yt.v, py[i2].v, G1[kd].v, ALU.mult)
        S.tt(xn[i2].v, xts[i2].v, yt.v, ALU.add)
        S.dma("pool", C.xd.v[t * 128:(t + 1) * 128, :], xn[i2].v)

    stage1(0)
    for it in range(len(tl_)):
        if it + 1 < len(tl_):
            stage1(it + 1)
        stage2(it)
    S.pop()


def even_mixer(S, C, l, ctx_out):
    S.push()
    HG = NS()
    HG.dec = S.sb("hg_dec", [128, 2, 4, 68], F32)
    HG.expm = S.sb("hg_expm", [128, 2, 4, 68], F32)
    S.push()
    hT = S.sb("hT_all", [128, 8, NTOK], BF16)
    even_E1(S, C, l, hT)
    hgrn_proj(S, C, l, hT, HG)
    mla_prep(S, C, l, hT)
    S.pop()
    hgrn_scan(S, C, l, ctx_out, HG)
    S.pop()
    attention(S, C, l, ctx_out)
    out_proj(S, C, l, ctx_out)


def build(layers=(0, 1, 2, 3), debug_out=()):
    nc = bass.Bass("TRN2", target_bir_lowering=False)
    S = Sched(nc)
    S.push()
    C = setup(S, debug_out)
    prologue(S, C, layers)
    for l in layers:
        ctx_out = l < 2
        if l % 2 == 0:
            even_mixer(S, C, l, ctx_out)
        else:
            odd_mixer(S, C, l, ctx_out)
        moe_layer(S, C, l, ctx_out)
    final_norm(S, C)
    S.pop()
    return nc, S


def make_in_maps(inp, consts):
    f = lambda a: np.ascontiguousarray(np.asarray(a, dtype=np.float32))
    shared = {
        "cctx_fm": f(np.asarray(inp["c_ctx"]).reshape(8, 128).T),
        "ada_w": f(inp["ada_w"]), "ada_b": f(inp["ada_b"]), "norm1_g": f(inp["norm1_g"]), "norm2_g": f(inp["norm2_g"]),
        "w_in": f(inp["w_in"]),
        "hg_lb_fm": f(np.asarray(inp["hg_lb"]).reshape(2, 2, 4, 128).transpose(3, 0, 1, 2).reshape(128, 16)),
        "hg_norm_g": f(inp["hg_norm_g"]),
        "mla_qn_g_fm": f(np.asarray(inp["mla_qn_g"]).reshape(2, 2, 128).transpose(2, 0, 1).reshape(128, 4)),
        "mla_wq_b": f(inp["mla_wq_b"]),
        "mla_kvn_g_fm": f(np.asarray(inp["mla_kvn_g"]).T),
        "mla_wkv_b": f(inp["mla_wkv_b"]), "w_out": f(inp["w_out"]), "pool_w": f(inp["pool_w"]),
        "pool_scale": f(inp["pool_scale"]), "router_w": f(inp["router_w"]),
        "exp_wg": f(inp["exp_wg"]), "exp_wu": f(inp["exp_wu"]), "exp_wd": f(inp["exp_wd"]),
        "final_g": f(np.asarray(inp["final_g"]).reshape(1, D)),
    }
    for k, v in consts.items():
        shared["k_" + k] = f(v).reshape(CONST_SHAPES[k])
    maps = []
    for core in range(8):
        b = core % 4
        m = dict(shared)
        m["x"] = f(inp["x"][b])
        m["ctx"] = f(inp["ctx"][b])
        m["c_fm"] = f(np.asarray(inp["c"][b]).reshape(8, 128).T)
        maps.append(m)
    return maps


def kernel(**inputs):
    nc, S = build()
    maps = make_in_maps(inputs, host_consts())
    res = run_bass_kernel_spmd(nc, maps, core_ids=list(range(8)))
    out = np.stack([np.asarray(res.results[b]["out"], dtype=np.float32) for b in range(4)], axis=0)
    return out
```

```python
import numpy as np
import ml_dtypes
from contextlib import ExitStack
import concourse.bass as bass
import concourse.mybir as mybir
from concourse.bass_utils import run_bass_kernel_spmd

F32 = mybir.dt.float32
BF16 = mybir.dt.bfloat16
I32 = mybir.dt.int32
AF = mybir.ActivationFunctionType
ALU = mybir.AluOpType
AX = mybir.AxisListType


class V:
    __slots__ = ("buf", "ap")

    def __init__(self, buf, ap):
        self.buf = buf
        self.ap = ap

    def __getitem__(self, idx):
        return V(self.buf, self.ap[idx])

    def re(self, pat, **kw):
        return V(self.buf, self.ap.rearrange(pat, **kw))

    def bc(self, shape):
        return V(self.buf, self.ap.to_broadcast(list(shape)))

    def pbc(self, n):
        return V(self.buf, self.ap.partition_broadcast(n))

    def us(self, axis):
        return V(self.buf, self.ap.unsqueeze(axis))

    def bitcast(self, dt):
        return V(self.buf, self.ap.bitcast(dt))


class Buf:
    def __init__(self, name, t, space):
        self.name = name
        self.t = t
        self.space = space
        self.lw = None
        self.rd = {}
        self.chan = None

    def __getitem__(self, idx):
        return V(self, self.t[idx])

    @property
    def v(self):
        return V(self, self.t[:] if self.space != "dram" else self.t)


class Chan:
    def __init__(self):
        self.sem = None
        self.cnt = 0


class Sched:
    SEM_MAX = 30000
    NCHAN = 20

    def __init__(self, nc):
        self.nc = nc
        self.E = {"pe": nc.tensor, "dve": nc.vector, "act": nc.scalar,
                  "pool": nc.gpsimd, "sp": nc.sync}
        self.sems = {}
        self.esem = {}
        self.ecnt = {}
        self.retired = []
        self.nsem = 0
        for k in self.E:
            self._new_esem(k)
        self.waited = {k: {} for k in self.E}
        self.chans = [Chan() for _ in range(self.NCHAN)]
        self.chan_rr = 0
        self.stacks = []
        self.ninst = 0

    def _alloc_sem(self, name):
        self.nsem += 1
        nm = "%s_%d" % (name, self.nsem)
        self.sems[nm] = self.nc.alloc_semaphore(name=nm)
        return nm

    def _new_esem(self, k):
        if k in self.esem and self.ecnt[k] > 0:
            self.retired.append((self.esem[k], self.ecnt[k]))
        self.esem[k] = self._alloc_sem("e" + k)
        self.ecnt[k] = 0

    def _wait(self, e, toks):
        need = {}
        for (n, v) in toks:
            if v > need.get(n, 0):
                need[n] = v
        w = self.waited[e]
        for n, v in need.items():
            if w.get(n, 0) >= v:
                continue
            self.E[e].wait_ge(self.sems[n], v)
            w[n] = v

    def push(self):
        self.stacks.append(ExitStack())

    def pop(self):
        self.barrier()
        self.stacks.pop().close()

    def sb(self, name, shape, dtype):
        self.nsem += 0
        self.uid = getattr(self, "uid", 0) + 1
        name = "%s_u%d" % (name, self.uid)
        t = self.stacks[-1].enter_context(self.nc.sbuf_tensor(name, list(shape), dtype))
        return Buf(name, t, "sbuf")

    def ps(self, name, shape, dtype=F32):
        self.uid = getattr(self, "uid", 0) + 1
        name = "%s_u%d" % (name, self.uid)
        t = self.stacks[-1].enter_context(self.nc.psum_tensor(name, list(shape), dtype))
        return Buf(name, t, "psum")

    def dram(self, name, shape, dtype, kind="Internal"):
        t = self.nc.dram_tensor(name, list(shape), dtype, kind=kind).ap()
        return Buf(name, t, "dram")

    def op(self, e, fn, reads=(), writes=()):
        own = self.esem[e]
        deps = []
        for x in reads:
            b = x.buf if isinstance(x, V) else x
            if b.lw is not None:
                deps.append(b.lw)
        for x in writes:
            b = x.buf if isinstance(x, V) else x
            if b.lw is not None and (b.lw[0] != own or e == "pool"):
                deps.append(b.lw)
            for n, v in b.rd.items():
                if n != own or e == "pool":
                    deps.append((n, v))
        self._wait(e, deps)
        inst = fn(self.E[e])
        if self.ecnt[e] >= self.SEM_MAX:
            self._new_esem(e)
        self.ecnt[e] += 1
        inst.then_inc(self.sems[self.esem[e]], 1)
        tok = (self.esem[e], self.ecnt[e])
        for x in reads:
            b = x.buf if isinstance(x, V) else x
            b.rd[tok[0]] = tok[1]
        for x in writes:
            b = x.buf if isinstance(x, V) else x
            b.lw = tok
            b.rd = {}
        self.ninst += 1
        return inst

    def _chan_of(self, b):
        if b.chan is None:
            b.chan = self.chans[self.chan_rr % self.NCHAN]
            self.chan_rr += 1
        return b.chan

    def dma(self, q, out, in_, owner=None, indirect=None, **kw):
        ob, ib = out.buf, in_.buf
        if owner is None:
            owner = ob if ob.space == "sbuf" else ib
        ch = self._chan_of(owner)
        if ch.sem is None or ch.cnt + 16 > self.SEM_MAX:
            if ch.sem is not None:
                self.retired.append((ch.sem, ch.cnt))
                self._wait(q, [(ch.sem, ch.cnt)])
            ch.sem = self._alloc_sem("d")
            ch.cnt = 0
        deps = []
        if ib.lw is not None:
            deps.append(ib.lw)
        if ob.lw is not None:
            deps.append(ob.lw)
        deps += list(ob.rd.items())
        extra_reads = kw.pop("extra_reads", ())
        for x in extra_reads:
            if x.buf.lw is not None:
                deps.append(x.buf.lw)
        if ch.cnt > 0:
            deps.append((ch.sem, ch.cnt))
        self._wait(q, deps)
        if indirect is None:
            inst = self.E[q].dma_start(out=out.ap, in_=in_.ap, **kw)
        else:
            inst = indirect(self.E[q])
        ch.cnt += 16
        inst.then_inc(self.sems[ch.sem], 16)
        tok = (ch.sem, ch.cnt)
        ob.lw = tok
        ob.rd = {}
        ib.rd[tok[0]] = tok[1]
        for x in extra_reads:
            x.buf.rd[tok[0]] = tok[1]
        self.ninst += 1
        return inst

    def barrier(self):
        toks = [(self.esem[k], self.ecnt[k]) for k in self.E if self.ecnt[k] > 0]
        toks += [(c.sem, c.cnt) for c in self.chans if c.sem is not None and c.cnt > 0]
        toks += self.retired
        for e in self.E:
            self._wait(e, toks)
        self.retired = []

    def mm(self, out, lhsT, rhs, start=True, stop=True):
        return self.op("pe", lambda e: e.matmul(out.ap, lhsT.ap, rhs.ap, start=start, stop=stop),
                       reads=(lhsT, rhs), writes=(out,))

    def tr(self, out, in_, ident):
        return self.op("pe", lambda e: e.transpose(out.ap, in_.ap, ident.ap),
                       reads=(in_, ident), writes=(out,))

    def act(self, out, in_, func, bias=None, scale=None, accum=None, eng="act"):
        kw = {}
        rd = [in_]
        wr = [out]
        if bias is not None:
            if isinstance(bias, V):
                kw["bias"] = bias.ap
                rd.append(bias)
            else:
                kw["bias"] = bias
        if scale is not None:
            if isinstance(scale, V):
                kw["scale"] = scale.ap
                rd.append(scale)
            else:
                kw["scale"] = scale
        if accum is not None:
            kw["accum_out"] = accum.ap
            wr.append(accum)
        return self.op("act", lambda e: e.activation(out.ap, in_.ap, func, **kw), reads=rd, writes=wr)

    def tt(self, out, a, b, op, eng="dve"):
        return self.op(eng, lambda e: e.tensor_tensor(out.ap, a.ap, b.ap, op), reads=(a, b), writes=(out,))

    def ts(self, out, a, s1, op0, s2=None, op1=None, accum=None, eng="dve"):
        rd = [a]
        wr = [out]
        a1 = s1
        a2 = s2
        if isinstance(s1, V):
            rd.append(s1)
            a1 = s1.ap
        if isinstance(s2, V):
            rd.append(s2)
            a2 = s2.ap
        kw = {}
        if op1 is not None:
            kw["op1"] = op1
        if accum is not None:
            kw["accum_out"] = accum.ap
            wr.append(accum)
        return self.op(eng, lambda e: e.tensor_scalar(out.ap, a.ap, a1, a2, op0, **kw), reads=rd, writes=wr)

    def stt(self, out, a, s, b, op0, op1):
        rd = [a, b]
        sv = s
        if isinstance(s, V):
            rd.append(s)
            sv = s.ap
        return self.op("dve", lambda e: e.scalar_tensor_tensor(out.ap, a.ap, sv, b.ap, op0, op1),
                       reads=rd, writes=(out,))

    def cp(self, out, in_, eng="dve"):
        if eng == "act":
            return self.op("act", lambda e: e.copy(out.ap, in_.ap), reads=(in_,), writes=(out,))
        return self.op(eng, lambda e: e.tensor_copy(out.ap, in_.ap), reads=(in_,), writes=(out,))

    def memset(self, out, val, eng="dve"):
        return self.op(eng, lambda e: e.memset(out.ap, val), writes=(out,))

    def red(self, out, in_, op, axis=AX.X):
        return self.op("dve", lambda e: e.tensor_reduce(out.ap, in_.ap, axis, op), reads=(in_,), writes=(out,))

    def recip(self, out, in_):
        return self.op("dve", lambda e: e.reciprocal(out.ap, in_.ap), reads=(in_,), writes=(out,))


D = 1024
NT = 34
NTOK = NT * 128
NLAT = 4096
NCTX = 256
EPS = 1e-6
NE = 16
FF = 2048
POOL_WINDOWS = (2, 4, 8, 16)
MLA_SCALE = (128 + 64) ** -0.5
O_Q, O_FFW, O_FBW, O_I, O_G, O_QA, O_KVA, O_KPE = 0, 512, 1024, 1536, 2048, 2560, 2816, 2944


class NS:
    pass


def host_consts():
    c = {}
    c["identf"] = np.eye(128, dtype=np.float32)
    c["onesf"] = np.ones((128, 128), np.float32)
    p = np.arange(128)
    c["lstrict"] = (p[:, None] < p[None, :]).astype(np.float32)
    c["iota"] = np.broadcast_to(np.arange(512, dtype=np.float32), (128, 512)).copy()
    c["tokid"] = (np.arange(NT)[None, :] * 128 + p[:, None]).astype(np.float32)
    c["toka"] = np.floor(c["tokid"] / 64.0).astype(np.float32)
    c["tokb"] = (c["tokid"] - 64.0 * c["toka"]).astype(np.float32)
    s = (p % 64)[:, None]
    t = np.arange(64)[None, :]
    c["trimask"] = np.stack([(s <= t), (s >= t)], axis=1).astype(np.float32)
    rm = np.ones((128, 512), np.float32)
    rm[:, ::64] = 0.0
    c["rmask"] = rm
    band = np.zeros((4, 5, 128, 128), np.float32)
    n = 3 * 128
    for gi, w in enumerate(POOL_WINDOWS):
        def full(nseq, tile):
            tt_ = np.arange(nseq)
            lo = np.clip(tt_ - w // 2, 0, nseq - 1)
            hi = np.clip(tt_ + w // 2 - 1, 0, nseq - 1)
            M = np.zeros((nseq, nseq), np.float64)
            for ti in range(nseq):
                M[lo[ti]:hi[ti] + 1, ti] = 1.0 / (hi[ti] - lo[ti] + 1)
                M[ti, ti] -= 1.0
            return M
        M = full(n, 1)
        band[gi, 0] = M[0:128, 128:256]
        band[gi, 1] = M[128:256, 128:256]
        band[gi, 2] = M[256:384, 128:256]
        band[gi, 3] = M[0:128, 0:128]
        band[gi, 4] = M[256:384, 256:384]
    c["band"] = np.ascontiguousarray(band.transpose(2, 0, 1, 3)).reshape(128, 20 * 128)
    rows = NLAT // 64
    row = np.repeat(np.arange(rows, dtype=np.float32), 64)
    col = np.tile(np.arange(64, dtype=np.float32), rows)
    half = 32
    inv = (1.0 / (np.float32(10000.0) ** (np.arange(0, half, 2, dtype=np.float32) / np.float32(half)))).astype(np.float32)
    ar = row[:, None] * inv[None, :]
    ac = col[:, None] * inv[None, :]
    ang = np.concatenate([ar, ar, ac, ac], axis=-1).astype(np.float32)
    cos = np.ones((64, NTOK), np.float32)
    sin = np.zeros((64, NTOK), np.float32)
    cos[:, NCTX:] = np.cos(ang).T
    sin[:, NCTX:] = np.sin(ang).T
    c["cosT"] = cos
    c["sinT"] = sin
    return c


CONST_SHAPES = {"identf": [128, 128], "onesf": [128, 128], "lstrict": [128, 128], "iota": [128, 512],
                "tokid": [128, NT], "toka": [128, NT], "tokb": [128, NT], "trimask": [128, 2, 64], "rmask": [128, 512], "band": [128, 2560],
                "cosT": [64, NTOK], "sinT": [64, NTOK]}

INPUT_SHAPES = {
    "x": [NLAT, D], "ctx": [NCTX, D], "c_fm": [128, 8], "cctx_fm": [128, 8],
    "ada_w": [4, D, 6 * D], "ada_b": [4, 6 * D], "norm1_g": [4, D], "norm2_g": [4, D],
    "w_in": [2, D, 3008], "hg_lb_fm": [128, 16], "hg_norm_g": [2, 128], "mla_qn_g_fm": [128, 4],
    "mla_wq_b": [2, 256, 768], "mla_kvn_g_fm": [128, 2], "mla_wkv_b": [2, 128, 1024], "w_out": [2, D, D],
    "pool_w": [2, 4, 256, 256], "pool_scale": [2, D], "router_w": [4, D, NE],
    "exp_wg": [4, NE, D, FF], "exp_wu": [4, NE, D, FF], "exp_wd": [4, NE, FF, D], "final_g": [1, D],
}


def setup(S, debug_out=()):
    C = NS()
    for k, shp in INPUT_SHAPES.items():
        setattr(C, k, S.dram(k, shp, F32, kind="ExternalInput"))
    for k, shp in CONST_SHAPES.items():
        setattr(C, "h_" + k, S.dram("k_" + k, shp, F32, kind="ExternalInput"))
    C.out = S.dram("out", [NLAT, D], F32, kind="ExternalOutput")
    C.xd = S.dram("xd", [NTOK, D], F32, kind="ExternalOutput" if "xd" in debug_out else "Internal")
    kd = lambda n: "ExternalOutput" if n in debug_out else "Internal"
    C.h2d = S.dram("h2d", [NTOK, D], BF16, kind=kd("h2d"))
    C.mixd = S.dram("mixd", [NTOK, D], BF16, kind=kd("mixd"))
    C.modtab = S.dram("modtab", [4, 2, 6, D], F32)
    C.knT_d = S.dram("knT_d", [128, 4, NTOK], BF16, kind=kd("knT_d"))
    C.kpeT_d = S.dram("kpeT_d", [64, NTOK], BF16, kind=kd("kpeT_d"))
    C.v_d = S.dram("v_d", [128, NT, 4, 130], BF16, kind=kd("v_d"))
    C.qnT_d = S.dram("qnT_d", [128, 2, NTOK], BF16, kind=kd("qnT_d"))
    C.qk_d = S.dram("qk_d", [2, 128, 4, 2, NTOK], BF16)
    C.khT_d = S.dram("khT_d", [2, 128, NT, 4, 128], BF16)
    C.vh_d = S.dram("vh_d", [128, NT, 512], BF16)
    C.sg_d = S.dram("sg_d", [128, NT, 512], BF16)
    C.identf = S.sb("identf", [128, 128], F32)
    C.identb = S.sb("identb", [128, 128], BF16)
    C.onesf = S.sb("onesf", [128, 128], F32)
    C.epsb = S.sb("epsb", [128, 1], F32)
    S.dma("sp", C.identf.v, C.h_identf.v)
    S.dma("sp", C.onesf.v, C.h_onesf.v)
    S.cp(C.identb.v, C.identf.v)
    S.memset(C.epsb.v, EPS)
    S.dma("sp", C.xd.v[0:NCTX, :], C.ctx.v, owner=C.identf)
    S.dma("sp", C.xd.v[NCTX:NTOK, :], C.x.v, owner=C.onesf)
    return C


def norm_mod(S, C, xt, A, Bv, out, W):
    S.act(W.junk.v, xt, AF.Square, accum=W.ssq.v)
    S.act(W.lnv.v, W.ssq.v, AF.Ln, scale=1.0 / D, bias=C.epsb.v)
    S.act(W.rstd.v, W.lnv.v, AF.Exp, scale=-0.5)
    S.stt(W.tmp.v, xt, W.rstd.v, A, ALU.mult, ALU.mult)
    S.tt(out, W.tmp.v, Bv, ALU.add)


def norm_ws(S, pfx):
    W = NS()
    W.junk = S.sb(pfx + "junk", [128, D], F32)
    W.tmp = S.sb(pfx + "tmp", [128, D], F32)
    W.ssq = S.sb(pfx + "ssq", [128, 1], F32)
    W.rstd = S.sb(pfx + "rstd", [128, 1], F32)
    W.lnv = S.sb(pfx + "lnv", [128, 1], F32)
    return W


def prologue(S, C, layers):
    S.push()
    cin = S.sb("cin", [128, 2, 8], F32)
    sc = S.sb("sc", [128, 2, 8], F32)
    S.dma("sp", cin[:, 0, :], C.c_fm.v)
    S.dma("sp", cin[:, 1, :], C.cctx_fm.v)
    S.act(sc.v, cin.v, AF.Silu)
    lhs2 = S.sb("lhs2", [128, 8, 2], F32)
    S.cp(lhs2.v, sc.v.re("p a k -> p k a"))
    g1 = S.sb("g1", [2, D], F32)
    g2 = S.sb("g2", [2, D], F32)
    psc = S.sb("psc", [2, D], F32)
    wb = [S.sb("adaw%d" % i, [128, 8, D], F32) for i in range(3)]
    brow = [S.sb("brow%d" % i, [1, D], F32) for i in range(3)]
    stg = [S.sb("stg%d" % i, [2, 512], F32) for i in range(2)]
    psb = [S.ps("pps%d" % i, [128, 512], F32) for i in range(2)]
    cnt = 0
    nld = 0
    for l in layers:
        S.dma("sp", g1.v, C.norm1_g.v[l:l + 1, :].pbc(2))
        S.dma("sp", g2.v, C.norm2_g.v[l:l + 1, :].pbc(2))
        if l % 2 == 1:
            S.dma("sp", psc.v, C.pool_scale.v[l // 2:l // 2 + 1, :].pbc(2))
        for seg in range(6):
            w = wb[nld % 3]
            br = brow[nld % 3]
            q = "sp" if nld % 2 == 0 else "act"
            nld += 1
            S.dma(q, w.v, C.ada_w.v[l].re("(k p) n -> p k n", p=128)[:, :, seg * D:(seg + 1) * D])
            S.dma(q, br.v, C.ada_b.v[l:l + 1, seg * D:(seg + 1) * D])
            for half in range(2):
                hs = slice(half * 512, (half + 1) * 512)
                ps = psb[cnt % 2]
                st = stg[cnt % 2]
                cnt += 1
                for k in range(8):
                    S.mm(ps[0:2, :], lhs2[:, k, :], w[:, k, hs], start=(k == 0), stop=False)
                S.mm(ps[0:2, :], C.onesf[0:1, 0:2], br[:, hs], start=False, stop=True)
                if seg == 1:
                    S.stt(st.v, ps[0:2, :], 1.0, g1[:, hs], ALU.add, ALU.mult)
                elif seg == 4:
                    S.stt(st.v, ps[0:2, :], 1.0, g2[:, hs], ALU.add, ALU.mult)
                elif seg == 2 and l % 2 == 1:
                    S.tt(st.v, ps[0:2, :], psc[:, hs], ALU.mult)
                else:
                    S.cp(st.v, ps[0:2, :])
                S.dma("pool", C.modtab.v[l, :, seg, hs], st.v)
    S.pop()


def load_mod(S, C, l, kind, seg, buf, q="sp"):
    S.dma(q, buf.v, C.modtab.v[l, kind, seg:seg + 1, :].pbc(128))


def moe_route(S, C, l, has_ctx, R):
    S.push()
    kinds = (0, 1) if has_ctx else (0,)
    A2 = {}
    B2 = {}
    for kd in kinds:
        A2[kd] = S.sb("A2_%d" % kd, [128, D], F32)
        B2[kd] = S.sb("B2_%d" % kd, [128, D], F32)
        load_mod(S, C, l, kd, 4, A2[kd])
        load_mod(S, C, l, kd, 3, B2[kd])
    rw = S.sb("rw", [128, 8, NE], F32)
    S.dma("sp", rw.v, C.router_w.v[l].re("(k p) e -> p k e", p=128))
    Ws = [norm_ws(S, "r%d" % i) for i in range(2)]
    xts = [S.sb("rxt%d" % i, [128, D], F32) for i in range(2)]
    h2f = [S.sb("h2f%d" % i, [128, D], F32) for i in range(2)]
    h2b = [S.sb("h2b%d" % i, [128, D], BF16) for i in range(2)]
    h2T = [S.sb("h2T%d" % i, [128, D], F32) for i in range(2)]
    ptr = [S.ps("rptr%d" % i, [128, D], F32) for i in range(2)]
    pl = [S.ps("rpl%d" % i, [128, 512], F32) for i in range(2)]
    mx = [S.sb("rmx%d" % i, [128, 1], F32) for i in range(2)]
    nmx = [S.sb("rnmx%d" % i, [128, 1], F32) for i in range(2)]
    sm = [S.sb("rsm%d" % i, [128, 1], F32) for i in range(2)]
    ex = [S.sb("rex%d" % i, [128, NE], F32) for i in range(2)]
    tiles = list(range(0 if has_ctx else 2, NT))

    def stage1(it):
        j = tiles[it]
        kd = 1 if j < 2 else 0
        i2 = it % 2
        xt = xts[i2]
        S.dma("sp", xt.v, C.xd.v[j * 128:(j + 1) * 128, :])
        hf = h2f[i2]
        hb = h2b[i2]
        norm_mod(S, C, xt.v, A2[kd].v, B2[kd].v, hf.v, Ws[i2])
        S.cp(hb.v, hf.v, eng="act")
        S.dma("pool", C.h2d.v[j * 128:(j + 1) * 128, :], hb.v)
        pt = ptr[i2]
        for k in range(8):
            S.tr(pt[:, k * 128:(k + 1) * 128], hf[:, k * 128:(k + 1) * 128], C.identf.v)

    def stage2(it):
        j = tiles[it]
        i2 = it % 2
        pt = ptr[i2]
        S.cp(h2T[i2][:, 0:512], pt[:, 0:512], eng="act")
        S.cp(h2T[i2][:, 512:1024], pt[:, 512:1024])
        for k in range(8):
            S.mm(pl[i2][:, 0:NE], h2T[i2][:, k * 128:(k + 1) * 128], rw[:, k, :], start=(k == 0), stop=(k == 7))
        S.red(mx[i2].v, pl[i2][:, 0:NE], ALU.max)
        S.ts(nmx[i2].v, mx[i2].v, -1.0, ALU.mult)
        S.act(ex[i2].v, pl[i2][:, 0:NE], AF.Exp, bias=nmx[i2].v, accum=sm[i2].v)
        S.recip(sm[i2].v, sm[i2].v)
        S.ts(R.aff[:, j, :], ex[i2].v, sm[i2].v, ALU.mult)

    stage1(0)
    for it in range(len(tiles)):
        if it + 1 < len(tiles):
            stage1(it + 1)
        stage2(it)
    S.pop()


def moe_bisect(S, C, R, specs):
    S.push()
    st = []
    for (j0, T, cap, pfx) in specs:
        o = NS()
        o.affv = R.aff[:, j0:j0 + T, :]
        o.T, o.cap = T, cap
        o.mid = S.sb(pfx + "mid", [128, NE], F32)
        o.dd = S.sb(pfx + "dd", [128, NE], F32)
        o.cntp = S.sb(pfx + "cntp", [128, NE], F32)
        o.cmp = S.sb(pfx + "cmp", [128, T, NE], F32)
        o.pc = S.ps(pfx + "pc", [128, 512], F32)
        o.lo = R.thr[pfx]
        S.memset(o.lo.v, 0.0)
        st.append(o)
    w = 0.5
    for it in range(30):
        for o in st:
            S.ts(o.mid.v, o.lo.v, w, ALU.add)
            S.tt(o.cmp.v, o.affv, o.mid.v.us(1).bc([128, o.T, NE]), ALU.is_ge)
            S.red(o.cntp.v, o.cmp.v.re("p t e -> p e t"), ALU.add)
            S.mm(o.pc[:, 0:NE], C.onesf.v, o.cntp.v)
            S.ts(o.dd.v, o.pc[:, 0:NE], float(o.cap), ALU.is_ge, w, ALU.mult)
            S.tt(o.lo.v, o.lo.v, o.dd.v, ALU.add)
        w *= 0.5
    S.pop()


def moe_topk(S, C, R, j0, T, cap, idx_out, gate_out, pfx):
    S.push()
    ncc = (cap + 127) // 128
    M = min(cap, 128)
    affv = R.aff[:, j0:j0 + T, :]
    lo = R.thr[pfx]
    mask = S.sb(pfx + "mask", [128, T, NE], F32)
    S.tt(mask.v, affv, lo.v.us(1).bc([128, T, NE]), ALU.is_ge)
    lstr = S.sb(pfx + "lstr", [128, 128], F32)
    S.dma("sp", lstr.v, C.h_lstrict.v)
    pw = S.ps(pfx + "pw", [128, 512], F32)
    ptot = S.ps(pfx + "ptot", [128, 512], F32)
    mflat = mask.v.re("p t e -> p (t e)")
    S.mm(pw[:, 0:T * NE], lstr.v, mflat)
    S.mm(ptot[:, 0:T * NE], C.onesf.v, mflat)
    totS = S.sb(pfx + "totS", [128, NE, T], F32)
    rsm = S.sb(pfx + "rsm", [128, NE, T], F32)
    offs = S.sb(pfx + "offs", [128, NE, T], F32)
    S.memset(totS.v, 0.0)
    S.memset(rsm.v, 1.0)
    S.memset(rsm[:, :, 0:1], 0.0)
    S.cp(totS[:, :, 1:T], ptot[:, 0:T * NE].re("p (t e) -> p e t", e=NE)[:, :, 0:T - 1])
    S.op("dve", lambda e: e.tensor_tensor_scan(offs.t[:].rearrange("p e t -> p (e t)"),
                                               rsm.t[:].rearrange("p e t -> p (e t)"),
                                               totS.t[:].rearrange("p e t -> p (e t)"),
                                               0.0, ALU.mult, ALU.add),
         reads=(rsm, totS), writes=(offs,))
    pos = S.sb(pfx + "pos", [128, T, NE], F32)
    S.tt(pos.v, pw[:, 0:T * NE].re("p (t e) -> p t e", e=NE), offs.v.re("p e t -> p t e"), ALU.add)
    BIG = 65536.0
    S.stt(pos.v, pos.v, -BIG, mask.v, ALU.add, ALU.mult)
    S.ts(pos.v, pos.v, BIG, ALU.add)
    tg = S.sb(pfx + "tg", [128, T, NE, 5], BF16)
    tka = S.sb(pfx + "tka", [128, NT], F32)
    tkb = S.sb(pfx + "tkb", [128, NT], F32)
    S.dma("sp", tka.v, C.h_toka.v)
    S.dma("sp", tkb.v, C.h_tokb.v)
    S.cp(tg[:, :, :, 0], tka[:, j0:j0 + T].us(2).bc([128, T, NE]))
    S.cp(tg[:, :, :, 1], tkb[:, j0:j0 + T].us(2).bc([128, T, NE]))
    r1 = S.sb(pfx + "r1", [128, T, NE], F32)
    r2 = S.sb(pfx + "r2", [128, T, NE], F32)
    S.cp(tg[:, :, :, 2], affv)
    S.tt(r1.v, affv, tg[:, :, :, 2], ALU.subtract)
    S.cp(tg[:, :, :, 3], r1.v)
    S.tt(r2.v, r1.v, tg[:, :, :, 3], ALU.subtract)
    S.cp(tg[:, :, :, 4], r2.v)
    iota32 = S.sb(pfx + "iota32", [128, 512], F32)
    S.dma("sp", iota32.v, C.h_iota.v)
    iota = S.sb(pfx + "iota", [128, 512], mybir.dt.int16)
    S.cp(iota.v, iota32.v)
    sel = [S.sb(pfx + "sel%d" % i, [128, 512], BF16) for i in range(4)]
    psI = [S.ps(pfx + "psI%d" % i, [128, 512], F32) for i in range(ncc)]
    t5 = [S.sb(pfx + "t5_%d" % i, [128, 5], F32) for i in range(4)]
    n = 0
    for e in range(NE):
        for jj in range(T):
            sb_ = sel[n % 4]
            n += 1
            S.ts(sb_[:, 0:cap], iota[:, 0:cap], pos[:, jj, e:e + 1], ALU.is_equal)
            for cc in range(ncc):
                S.mm(psI[cc][0:M, 0:5], sb_[:, cc * M:(cc + 1) * M], tg[:, jj, e, :],
                     start=(jj == 0), stop=(jj == T - 1))
        for cc in range(ncc):
            t_ = t5[cc]
            S.cp(t_[0:M, :], psI[cc][0:M, 0:5], eng="act")
            S.stt(idx_out[0:M, e, cc:cc + 1], t_[0:M, 0:1], 64.0, t_[0:M, 1:2], ALU.mult, ALU.add)
            S.red(gate_out[0:M, e, cc:cc + 1], t_[0:M, 2:5], ALU.add)
    S.pop()


def moe_experts(S, C, l, has_ctx, R):
    S.push()
    G5 = S.sb("G5", [128, D], F32)
    load_mod(S, C, l, 0, 5, G5)
    if has_ctx:
        G5c = S.sb("G5c", [128, D], F32)
        load_mod(S, C, l, 1, 5, G5c)
    NL = 512
    NC_ = 32 if has_ctx else 0
    NTOKE = NL + NC_
    wgb = [S.sb("wgb%d" % i, [128, 8, 512], BF16) for i in range(3)]
    wub = [S.sb("wub%d" % i, [128, 8, 512], BF16) for i in range(3)]
    wdb = [S.sb("wdb%d" % i, [128, 16, 512], BF16) for i in range(2)]
    xs = [S.sb("xs%d" % i, [128, 5, D], BF16) for i in range(2)]
    xsT = S.sb("xsT", [128, 8, 544], BF16)
    hidT = S.sb("hidT", [128, 16, 544], BF16)
    sg = [S.sb("sg%d" % i, [128, 544], F32) for i in range(2)]
    ost = [S.sb("ost%d" % i, [128, 5, D], F32) for i in range(2)]
    pts = [S.ps("ept%d" % i, [128, D], BF16) for i in range(2)]
    pb = [S.ps("epb%d" % i, [128, 512], F32) for i in range(6)]

    def load_gu(i):
        e, fb = divmod(i, 4)
        b = i % 3
        S.dma("pool", wgb[b].v, C.exp_wg.v[l, e].re("(k p) f -> p k f", p=128)[:, :, fb * 512:(fb + 1) * 512])
        S.dma("pool", wub[b].v, C.exp_wu.v[l, e].re("(k p) f -> p k f", p=128)[:, :, fb * 512:(fb + 1) * 512])

    def load_d(i):
        e, dh = divmod(i, 2)
        S.dma("pool", wdb[i % 2].v, C.exp_wd.v[l, e].re("(k p) d -> p k d", p=128)[:, :, dh * 512:(dh + 1) * 512])

    def gather(e):
        x_ = xs[e % 2]
        for cc in range(4):
            S.dma("pool", x_[:, cc, :], C.h2d.v, extra_reads=(R.idx.v,),
                  indirect=lambda en, cc=cc: en.indirect_dma_start(
                      out=x_.t[:, cc, :], out_offset=None, in_=C.h2d.t[:, :],
                      in_offset=bass.IndirectOffsetOnAxis(ap=R.idx.t[:, e, cc:cc + 1], axis=0)))
        if has_ctx:
            S.dma("pool", x_[0:32, 4, :], C.h2d.v, extra_reads=(R.idxc.v,),
                  indirect=lambda en: en.indirect_dma_start(
                      out=x_.t[0:32, 4, :], out_offset=None, in_=C.h2d.t[:, :],
                      in_offset=bass.IndirectOffsetOnAxis(ap=R.idxc.t[0:32, e, 0:1], axis=0)))

    pend_scatter = []

    def emit_scatter(e, o_):
        for cc in range(4):
            S.dma("pool", C.xd.v, o_[:, cc, :], owner=o_, extra_reads=(R.idx.v,),
                  indirect=lambda en, cc=cc, o_=o_: en.indirect_dma_start(
                      out=C.xd.t[:, :], out_offset=bass.IndirectOffsetOnAxis(ap=R.idx.t[:, e, cc:cc + 1], axis=0),
                      in_=o_.t[:, cc, :], in_offset=None, compute_op=ALU.add))
        if has_ctx:
            S.dma("pool", C.xd.v, o_[0:32, 4, :], owner=o_, extra_reads=(R.idxc.v,),
                  indirect=lambda en, o_=o_: en.indirect_dma_start(
                      out=C.xd.t[:, :], out_offset=bass.IndirectOffsetOnAxis(ap=R.idxc.t[0:32, e, 0:1], axis=0),
                      in_=o_.t[0:32, 4, :], in_offset=None, compute_op=ALU.add))

    gather(0)
    load_gu(0)
    load_gu(1)
    load_d(0)
    for e in range(NE):
        x_ = xs[e % 2]
        for cc in range(4):
            pt = pts[cc % 2]
            for k in range(8):
                S.tr(pt[:, k * 128:(k + 1) * 128], x_[:, cc, k * 128:(k + 1) * 128], C.identb.v)
            S.cp(xsT[:, :, cc * 128:(cc + 1) * 128], pt.v.re("p (k t) -> p k t", k=8), eng=("act" if cc % 2 else "dve"))
        if has_ctx:
            pt = pts[0]
            for k in range(8):
                S.tr(pt[:, k * 128:k * 128 + 32], x_[0:32, 4, k * 128:(k + 1) * 128], C.identb[0:32, 0:32])
            S.cp(xsT[:, :, 512:544], pt.v.re("p (k t) -> p k t", k=8)[:, :, 0:32])
        if e + 1 < NE:
            gather(e + 1)
        for fb in range(4):
            gi = e * 4 + fb
            if gi + 2 < NE * 4:
                load_gu(gi + 2)
            wg_, wu_ = wgb[gi % 3], wub[gi % 3]
            for fs in range(4):
                fc = fb * 4 + fs
                pg, pu = pb[(fc % 2) * 2], pb[(fc % 2) * 2 + 1]
                for k in range(8):
                    S.mm(pg.v, wg_[:, k, fs * 128:(fs + 1) * 128], xsT[:, k, 0:512], start=(k == 0), stop=(k == 7))
                for k in range(8):
                    S.mm(pu.v, wu_[:, k, fs * 128:(fs + 1) * 128], xsT[:, k, 0:512], start=(k == 0), stop=(k == 7))
                s_ = sg[fc % 2]
                S.act(s_[:, 0:512], pg.v, AF.Silu)
                S.tt(hidT[:, fc, 0:512], s_[:, 0:512], pu.v, ALU.mult)
                if has_ctx:
                    pgc, puc = pb[4], pb[5]
                    for k in range(8):
                        S.mm(pgc[:, 0:32], wg_[:, k, fs * 128:(fs + 1) * 128], xsT[:, k, 512:544], start=(k == 0), stop=(k == 7))
                    for k in range(8):
                        S.mm(puc[:, 0:32], wu_[:, k, fs * 128:(fs + 1) * 128], xsT[:, k, 512:544], start=(k == 0), stop=(k == 7))
                    S.act(s_[:, 512:544], pgc[:, 0:32], AF.Silu)
                    S.tt(hidT[:, fc, 512:544], s_[:, 512:544], puc[:, 0:32], ALU.mult)
        while pend_scatter:
            emit_scatter(*pend_scatter.pop(0))
        o_ = ost[e % 2]
        for dh in range(2):
            di = e * 2 + dh
            if di + 1 < NE * 2:
                load_d(di + 1)
            wd_ = wdb[di % 2]
            ds = slice(dh * 512, (dh + 1) * 512)
            for cc in range(4):
                po = pb[cc]
                for fk in range(16):
                    S.mm(po.v, hidT[:, fk, cc * 128:(cc + 1) * 128], wd_[:, fk, :], start=(fk == 0), stop=(fk == 15))
                S.stt(o_[:, cc, ds], po.v, R.gate[:, e, cc:cc + 1], G5[:, ds], ALU.mult, ALU.mult)
            if has_ctx:
                po = pb[4]
                for fk in range(16):
                    S.mm(po[0:32, :], hidT[:, fk, 512:544], wd_[:, fk, :], start=(fk == 0), stop=(fk == 15))
                S.stt(o_[0:32, 4, ds], po[0:32, :], R.gatec[0:32, e, 0:1], G5c[0:32, ds], ALU.mult, ALU.mult)
        pend_scatter.append((e, o_))
    while pend_scatter:
        emit_scatter(*pend_scatter.pop(0))
    S.pop()


def moe_layer(S, C, l, has_ctx):
    S.push()
    R = NS()
    R.aff = S.sb("aff", [128, NT, NE], F32)
    R.idx = S.sb("idx", [128, NE, 4], I32)
    R.gate = S.sb("gate", [128, NE, 4], F32)
    R.idxc = S.sb("idxc", [128, NE, 1], I32)
    R.gatec = S.sb("gatec", [128, NE, 1], F32)
    R.thr = {"tl": S.sb("thr_l", [128, NE], F32), "tc": S.sb("thr_c", [128, NE], F32)}
    moe_route(S, C, l, has_ctx, R)
    moe_bisect(S, C, R, [(2, 32, 512, "tl")] + ([(0, 2, 32, "tc")] if has_ctx else []))
    moe_topk(S, C, R, 2, 32, 512, R.idx.v, R.gate.v, "tl")
    if has_ctx:
        moe_topk(S, C, R, 0, 2, 32, R.idxc.v, R.gatec.v, "tc")
    moe_experts(S, C, l, has_ctx, R)
    S.pop()


def odd_mixer(S, C, l, has_ctx):
    S.push()
    j = l // 2
    kinds = (0, 1) if has_ctx else (0,)
    A1, B1, G1 = {}, {}, {}
    for kd in kinds:
        A1[kd] = S.sb("A1_%d" % kd, [128, D], F32)
        B1[kd] = S.sb("B1_%d" % kd, [128, D], F32)
        G1[kd] = S.sb("G1_%d" % kd, [128, D], F32)
        load_mod(S, C, l, kd, 1, A1[kd])
        load_mod(S, C, l, kd, 0, B1[kd])
        load_mod(S, C, l, kd, 2, G1[kd])
    band = S.sb("band", [128, 4, 5, 128], F32)
    S.dma("sp", band.v.re("p g v t -> p (g v t)"), C.h_band.v)
    wp = S.sb("wp", [128, 8, 256], BF16)
    S.dma("pool", wp.v, C.pool_w.v[j].re("g (h p) d -> p (g h) d", p=128))
    Ws = [norm_ws(S, "o%d" % i) for i in range(2)]
    xts = [S.sb("oxt%d" % i, [128, D], F32) for i in range(4)]
    hs = [S.sb("oh%d" % i, [128, D], F32) for i in range(4)]
    ppT = S.sb("ppT", [128, 8, 128], BF16)
    yt = S.sb("oyt", [128, D], F32)
    xn = [S.sb("oxn%d" % i, [128, D], F32) for i in range(2)]
    pp = S.ps("opp", [128, D], F32)
    py = S.ps("opy", [128, D], F32)

    def prep(t):
        kd = 1 if t < 2 else 0
        S.dma("sp", xts[t % 4].v, C.xd.v[t * 128:(t + 1) * 128, :])
        norm_mod(S, C, xts[t % 4].v, A1[kd].v, B1[kd].v, hs[t % 4].v, Ws[t % 2])

    seqs = ([(0, 2)] if has_ctx else []) + [(2, NT)]
    for (t0, t1) in seqs:
        prep(t0)
        for t in range(t0, t1):
            if t + 1 < t1:
                prep(t + 1)
            kd = 1 if t < 2 else 0
            first, last = (t == t0), (t == t1 - 1)
            for ci in range(8):
                g = ci // 2
                terms = []
                if not first:
                    terms.append((t - 1, 0))
                terms.append((t, 3 if first else (4 if last else 1)))
                if not last:
                    terms.append((t + 1, 2))
                for n_, (tj, var) in enumerate(terms):
                    S.mm(pp[:, ci * 128:(ci + 1) * 128], hs[tj % 4][:, ci * 128:(ci + 1) * 128], band[:, g, var, :],
                         start=(n_ == 0), stop=(n_ == len(terms) - 1))
            S.cp(ppT.v.re("p c t -> p (c t)"), pp.v, eng="act")
            for g in range(4):
                for hf in range(2):
                    S.mm(py[:, g * 256:(g + 1) * 256], ppT[:, g * 2 + hf, :], wp[:, g * 2 + hf, :],
                         start=(hf == 0), stop=(hf == 1))
            S.tt(yt.v, py.v, G1[kd].v, ALU.mult)
            xo = xn[t % 2]
            S.tt(xo.v, xts[t % 4].v, yt.v, ALU.add)
            S.dma("pool", C.xd.v[t * 128:(t + 1) * 128, :], xo.v)
    S.pop()


def final_norm(S, C):
    S.push()
    gf = S.sb("gf", [128, D], F32)
    S.dma("sp", gf.v, C.final_g.v.pbc(128))
    Ws = [norm_ws(S, "f%d" % i) for i in range(2)]
    xts = [S.sb("fxt%d" % i, [128, D], F32) for i in range(2)]
    ots = [S.sb("fot%d" % i, [128, D], F32) for i in range(2)]
    for t in range(2, NT):
        xt = xts[t % 2]
        S.dma("sp", xt.v, C.xd.v[t * 128:(t + 1) * 128, :])
        W = Ws[t % 2]
        S.act(W.junk.v, xt.v, AF.Square, accum=W.ssq.v)
        S.act(W.lnv.v, W.ssq.v, AF.Ln, scale=1.0 / D, bias=C.epsb.v)
        S.act(W.rstd.v, W.lnv.v, AF.Exp, scale=-0.5)
        S.stt(ots[t % 2].v, xt.v, W.rstd.v, gf.v, ALU.mult, ALU.mult)
        S.dma("pool", C.out.v[(t - 2) * 128:(t - 1) * 128, :], ots[t % 2].v)
    S.pop()


BLOCKS = [(0, 256)] + [(256 + 512 * i, 512) for i in range(8)]


def even_E1(S, C, l, hT):
    S.push()
    A1, B1 = {}, {}
    for kd in (0, 1):
        A1[kd] = S.sb("eA1_%d" % kd, [128, D], F32)
        B1[kd] = S.sb("eB1_%d" % kd, [128, D], F32)
        load_mod(S, C, l, kd, 1, A1[kd])
        load_mod(S, C, l, kd, 0, B1[kd])
    Ws = [norm_ws(S, "e%d" % i) for i in range(2)]
    xts = [S.sb("ext%d" % i, [128, D], F32) for i in range(2)]
    hb = [S.sb("ehb%d" % i, [128, D], BF16) for i in range(2)]
    pt = [S.ps("ept%d" % i, [128, D], BF16) for i in range(2)]
    def stage1(t):
        kd = 1 if t < 2 else 0
        xt = xts[t % 2]
        S.dma("sp", xt.v, C.xd.v[t * 128:(t + 1) * 128, :])
        norm_mod(S, C, xt.v, A1[kd].v, B1[kd].v, hb[t % 2].v, Ws[t % 2])

    def stage2(t):
        for k in range(8):
            S.tr(pt[t % 2][:, k * 128:(k + 1) * 128], hb[t % 2][:, k * 128:(k + 1) * 128], C.identb.v)
        S.cp(hT[:, :, t * 128:(t + 1) * 128], pt[t % 2].v.re("p (k t) -> p k t", k=8), eng="act")

    stage1(0)
    for t in range(NT):
        if t + 1 < NT:
            stage1(t + 1)
        stage2(t)
    S.pop()


def hgrn_proj(S, C, l, hT, HG):
    j = l // 2
    S.push()
    rmask = S.sb("rmask", [128, 512], F32)
    S.dma("sp", rmask.v, C.h_rmask.v)
    lbin = S.sb("lbin", [128, 16], F32)
    S.dma("sp", lbin.v, C.hg_lb_fm.v)
    lbv = S.sb("lbv", [128, 8], F32)
    oml = S.sb("oml", [128, 8], F32)
    if j == 0:
        S.memset(lbv.v, 0.0)
    else:
        S.tt(lbv.v, lbin[:, 8:16], lbin[:, 0:8], ALU.subtract)
        S.act(lbv.v, lbv.v, AF.Sigmoid)
    S.ts(oml.v, lbv.v, -1.0, ALU.mult, 1.0, ALU.add)
    wall = {}
    for nm, off in (("q", O_Q), ("zf", O_FFW), ("zb", O_FBW), ("i", O_I), ("g", O_G)):
        wall[nm] = S.sb("wa_" + nm, [128, 8, 512], BF16)
        S.dma("pool", wall[nm].v, C.w_in.v[j].re("(k p) c -> p k c", p=128)[:, :, off:off + 512])
    wks = [{nm: S.sb("hw%d_" % i + nm, [128, 512], F32) for nm in ("q32", "s32", "lf", "k32", "cb", "d1", "d2", "eq", "ek", "ef", "eh", "ez", "den")}
           for i in range(2)]
    qkb = [[S.sb("qkb%d_%d" % (d, i), [128, 2, 512], BF16) for i in range(2)] for d in range(2)]
    khb = [S.sb("khb%d" % i, [128, 512], BF16) for i in range(2)]
    khT = [S.sb("khTs%d" % i, [128, 4, 128], BF16) for i in range(2)]
    vst = [S.sb("vst%d" % i, [128, 512], BF16) for i in range(2)]
    gst = [S.sb("gst%d" % i, [128, 512], BF16) for i in range(2)]
    pj = [S.ps("hpj%d" % i, [128, 512], F32) for i in range(3)]
    pvg = [S.ps("hpvg%d" % i, [128, 512], F32) for i in range(2)]
    ptb = [S.ps("hptb%d" % i, [128, 512], BF16) for i in range(2)]
    for t in range(NT):
        for k in range(8):
            S.mm(pvg[0].v, hT[:, k, t * 128:(t + 1) * 128], wall["i"][:, k, :], start=(k == 0), stop=(k == 7))
        for k in range(8):
            S.mm(pvg[1].v, hT[:, k, t * 128:(t + 1) * 128], wall["g"][:, k, :], start=(k == 0), stop=(k == 7))
        S.cp(vst[t % 2].v, pvg[0].v)
        S.act(gst[t % 2].v, pvg[1].v, AF.Silu)
        S.dma("sp", C.vh_d.v[:, t, :], vst[t % 2].v)
        S.dma("sp", C.sg_d.v[:, t, :], gst[t % 2].v)
    n_it = 0
    for h in range(4):
        hs = slice(h * 128, (h + 1) * 128)
        for (t0, n) in BLOCKS:
            nch = n // 64
            c0 = t0 // 64
            ts_ = slice(t0, t0 + n)
            pq = pj[0]
            for k in range(8):
                S.mm(pq[:, 0:n], wall["q"][:, k, hs], hT[:, k, ts_], start=(k == 0), stop=(k == 7))
            q32 = wks[n_it % 2]["q32"]
            S.cp(q32[:, 0:n], pq[:, 0:n], eng="act")
            mcols = (31, 32)
            lasts = (63, 0)
            for d in range(2):
                wk = wks[d]
                wz = wall["zf"] if d == 0 else wall["zb"]
                pz = pj[1 + d]
                for k in range(8):
                    S.mm(pz[:, 0:n], wz[:, k, hs], hT[:, k, ts_], start=(k == 0), stop=(k == 7))
                S.act(wk["ez"][:, 0:n], pz[:, 0:n], AF.Exp, scale=-1.0)
                S.ts(wk["den"][:, 0:n], wk["ez"][:, 0:n], 1.0, ALU.add)
                S.recip(wk["s32"][:, 0:n], wk["den"][:, 0:n])
                if j != 0:
                    S.ts(wk["s32"][:, 0:n], wk["s32"][:, 0:n], oml[:, d * 4 + h:d * 4 + h + 1], ALU.mult,
                         lbv[:, d * 4 + h:d * 4 + h + 1], ALU.add)
            for d in range(2):
                wk = wks[d]
                S.act(wk["lf"][:, 0:n], wk["s32"][:, 0:n], AF.Ln)
                S.ts(wk["k32"][:, 0:n], wk["s32"][:, 0:n], -1.0, ALU.mult, 1.0, ALU.add, eng="pool")
            for d in range(2):
                wk = wks[d]
                lf, cb, d1, d2 = wk["lf"], wk["cb"], wk["d1"], wk["d2"]
                S.op("dve", lambda e, n=n, cb=cb, lf=lf: e.tensor_tensor_scan(cb.t[:, 0:n], rmask.t[:, 0:n], lf.t[:, 0:n],
                                                                              0.0, ALU.mult, ALU.add),
                     reads=(rmask, lf), writes=(cb,))
                cb3 = cb[:, 0:n].re("p (c s) -> p c s", s=64)
                d13 = d1[:, 0:n].re("p (c s) -> p c s", s=64)
                d23 = d2[:, 0:n].re("p (c s) -> p c s", s=64)
                if d == 1:
                    S.tt(d13, cb3[:, :, 63:64].bc([128, nch, 64]), cb3, ALU.subtract)
                    S.tt(cb[:, 0:n], d1[:, 0:n], lf[:, 0:n], ALU.add)
                mcol, last = mcols[d], lasts[d]
                S.tt(d13, cb3, cb3[:, :, mcol:mcol + 1].bc([128, nch, 64]), ALU.subtract)
                S.tt(d23, cb3, cb3[:, :, last:last + 1].bc([128, nch, 64]), ALU.subtract)
            for d in range(2):
                wk = wks[d]
                S.act(wk["eq"][:, 0:n], wk["d1"][:, 0:n], AF.Exp)
                S.act(wk["ek"][:, 0:n], wk["d1"][:, 0:n], AF.Exp, scale=-1.0)
                S.act(wk["ef"][:, 0:n], wk["cb"][:, 0:n], AF.Exp)
                S.act(wk["eh"][:, 0:n], wk["d2"][:, 0:n], AF.Exp, scale=-1.0)
            for d in range(2):
                wk = wks[d]
                k32, eq, ek, ef, eh = wk["k32"], wk["eq"], wk["ek"], wk["ef"], wk["eh"]
                mcol, last = mcols[d], lasts[d]
                qb = qkb[d][n_it % 2]
                S.tt(qb[:, 0, 0:n], q32[:, 0:n], eq[:, 0:n], ALU.mult)
                S.tt(qb[:, 1, 0:n], k32[:, 0:n], ek[:, 0:n], ALU.mult, eng="pool")
                kh_ = khb[d]
                S.tt(kh_[:, 0:n], k32[:, 0:n], eh[:, 0:n], ALU.mult, eng="pool")
                ef3 = ef[:, 0:n].re("p (c s) -> p c s", s=64)
                S.cp(HG.dec[:, d, h, c0:c0 + nch], ef3[:, :, last])
                S.cp(HG.expm[:, d, h, c0:c0 + nch], ef3[:, :, mcol])
                S.dma("sp", C.qk_d.v[d][:, h, :, ts_], qb[:, :, 0:n])
                pt_ = ptb[d]
                kt_s = khT[d]
                ntl = n // 128
                for i in range(ntl):
                    S.tr(pt_[:, i * 128:(i + 1) * 128], kh_[:, i * 128:(i + 1) * 128], C.identb.v)
                S.cp(kt_s[:, 0:ntl, :], pt_[:, 0:n].re("p (t k) -> p t k", k=128), eng="act")
                S.dma("sp", C.khT_d.v[d][:, t0 // 128:t0 // 128 + ntl, h, :], kt_s[:, 0:ntl, :])
            n_it += 1
    S.pop()


def hgrn_scan(S, C, l, ctx_out, HG):
    j = l // 2
    S.push()
    v_all = S.sb("v_all", [128, NT, 512], BF16)
    S.dma("sp", v_all.v, C.vh_d.v)
    oacc = S.sb("oacc", [128, NT, 512], F32)
    S.memset(oacc.v, 0.0, eng="pool")
    trimf = S.sb("trimf", [128, 2, 64], F32)
    S.dma("sp", trimf.v, C.h_trimask.v)
    maskI = S.sb("maskI", [128, 2, 4, 64], I32)
    for h in range(4):
        S.cp(maskI[:, :, h, :], trimf.v)
    hgg = S.sb("hgg", [128, 128], F32)
    S.dma("sp", hgg.v, C.hg_norm_g.v[j:j + 1, :].pbc(128))
    Sf = [[S.sb("Sf%d_%d" % (d, h), [128, 128], F32) for h in range(4)] for d in range(2)]
    Sb = [[S.sb("Sb%d_%d" % (d, h), [128, 128], BF16) for h in range(4)] for d in range(2)]
    attT = [S.sb("attT%d" % d, [128, 4, 64], BF16) for d in range(2)]
    qkblk = [[S.sb("qkblk%d_%d" % (d, i), [128, 4, 2, 512], BF16) for i in range(2)] for d in range(2)]
    khblk = [[S.sb("khblk%d_%d" % (d, i), [128, 4, 4, 128], BF16) for i in range(2)] for d in range(2)]
    psA = [S.ps("hpsA%d" % d, [128, 512], F32) for d in range(2)]
    psO = [S.ps("hpsO%d" % d, [128, 512], F32) for d in range(2)]
    psU = [S.ps("hpsU%d" % d, [128, 512], F32) for d in range(2)]
    for d in range(2):
        for h in range(4):
            S.memset(Sf[d][h].v, 0.0)
            S.memset(Sb[d][h].v, 0.0)
        S.memset(attT[d].v, 0.0)
    bseq = [list(range(9)), [0] + list(range(8, 0, -1))]
    order = [list(range(68)), [3, 2, 1, 0] + list(range(67, 3, -1))]
    dq = ["sp", "pool"]

    def load_block(d, si):
        b = bseq[d][si]
        t0, n = BLOCKS[b]
        S.dma(dq[d], qkblk[d][si % 2][:, :, :, 0:n], C.qk_d.v[d][:, :, :, t0:t0 + n])
        S.dma(dq[d], khblk[d][si % 2][:, 0:n // 128, :, :], C.khT_d.v[d][:, t0 // 128:t0 // 128 + n // 128, :, :])

    cur_si = [-1, -1]
    for d in range(2):
        load_block(d, 0)
        load_block(d, 1)
    for step in range(68):
        inf = []
        for d in range(2):
            c = order[d][step]
            b = 0 if c < 4 else 1 + (c - 4) // 8
            si = bseq[d].index(b)
            if si != cur_si[d]:
                cur_si[d] = si
                if si >= 1 and si + 1 < 9:
                    load_block(d, si + 1)
            t0b, nb = BLOCKS[b]
            lc = c - t0b // 64
            o = NS()
            o.c = c
            o.ls = slice(lc * 64, lc * 64 + 64)
            o.tl = (c // 2) - t0b // 128
            o.t = c // 2
            pb_ = (c % 2) * 64
            o.rows = slice(pb_, pb_ + 64)
            o.qk = qkblk[d][si % 2]
            o.kh = khblk[d][si % 2]
            o.out = ctx_out or c >= 4
            inf.append(o)
        for d in range(2):
            o = inf[d]
            if o.out:
                for h in range(4):
                    S.mm(psA[d][o.rows, h * 64:(h + 1) * 64], o.qk[:, h, 1, o.ls], o.qk[:, h, 0, o.ls])
                S.op("dve", lambda e, d=d, rows=o.rows: e.copy_predicated(
                    attT[d].t[rows, :, :], maskI.t[rows, d, :, :],
                    psA[d].t[rows, 0:256].rearrange("p (h t) -> p h t", h=4)),
                    reads=(maskI, psA[d]), writes=(attT[d],))
        if step < 67:
            for d in range(2):
                o = inf[d]
                for h in range(4):
                    hs = slice(h * 128, (h + 1) * 128)
                    S.mm(psU[d][:, hs], o.kh[o.rows, o.tl, h, :], v_all[o.rows, o.t, hs])
        for d in range(2):
            o = inf[d]
            if o.out:
                for h in range(4):
                    hs = slice(h * 128, (h + 1) * 128)
                    S.mm(psO[d][o.rows, hs], attT[d][o.rows, h, :], v_all[o.rows, o.t, hs], start=True, stop=False)
                    S.mm(psO[d][o.rows, hs], o.qk[:, h, 0, o.ls], Sb[d][h].v, start=False, stop=True)
                S.tt(oacc[o.rows, o.t, :], oacc[o.rows, o.t, :], psO[d][o.rows, :], ALU.add)
        if step < 67:
            for d in range(2):
                o = inf[d]
                cn = order[d][step + 1]
                for h in range(4):
                    hs = slice(h * 128, (h + 1) * 128)
                    S.stt(Sf[d][h].v, Sf[d][h].v, HG.dec[:, d, h, o.c:o.c + 1], psU[d][:, hs], ALU.mult, ALU.add)
                    S.act(Sb[d][h].v, Sf[d][h].v, AF.Identity, scale=HG.expm[:, d, h, cn:cn + 1])
    tmin = 0 if ctx_out else 2
    sq = S.sb("hsq", [128, 4, 512], F32)
    ss = S.sb("hss", [128, 16], F32)
    sgb = [S.sb("hsgb%d" % i, [128, 4, 512], BF16) for i in range(2)]
    hgo = [S.sb("hgo%d" % i, [128, 4, 512], BF16) for i in range(2)]
    mv = C.mixd.v.re("(t p) f -> p t f", p=128)
    for gi, t4 in enumerate(range(tmin, NT, 4)):
        n4 = min(4, NT - t4)
        o4 = oacc[:, t4:t4 + n4, :]
        S.dma("sp", sgb[gi % 2][:, 0:n4, :], C.sg_d.v[:, t4:t4 + n4, :])
        S.tt(sq[:, 0:n4, :], o4, o4, ALU.mult)
        S.red(ss[:, 0:n4 * 4], sq[:, 0:n4, :].re("p t (h v) -> p (t h) v", h=4), ALU.add)
        S.act(ss[:, 0:n4 * 4], ss[:, 0:n4 * 4], AF.Sqrt, scale=1.0 / 128, bias=C.epsb.v)
        S.recip(ss[:, 0:n4 * 4], ss[:, 0:n4 * 4])
        o4h = o4.re("p t (h v) -> p (t h) v", h=4)
        S.tt(o4h, o4h, ss[:, 0:n4 * 4].us(2).bc([128, n4 * 4, 128]), ALU.mult)
        S.tt(o4h, o4h, hgg.v.us(1).bc([128, n4 * 4, 128]), ALU.mult)
        S.tt(hgo[gi % 2][:, 0:n4, :], o4, sgb[gi % 2][:, 0:n4, :], ALU.mult)
        S.dma("pool", mv[:, t4:t4 + n4, 0:512], hgo[gi % 2][:, 0:n4, :])
    S.pop()


def mla_prep(S, C, l, hT):
    j = l // 2
    S.push()
    wm = S.sb("wm", [128, 8, 448], BF16)
    S.dma("pool", wm.v, C.w_in.v[j].re("(k p) c -> p k c", p=128)[:, :, O_QA:3008])
    wmrot = S.sb("wmrot", [128, 8, 64], BF16)
    pe5 = wm[:, :, 384:448].re("p k (a two s) -> p k a two s", a=2, two=2)
    ro5 = wmrot.v.re("p k (a two s) -> p k a two s", a=2, two=2)
    S.ts(ro5[:, :, :, 0, :], pe5[:, :, :, 1, :], -1.0, ALU.mult)
    S.cp(ro5[:, :, :, 1, :], pe5[:, :, :, 0, :])
    wkvb = S.sb("wkvb", [128, 1024], BF16)
    S.dma("pool", wkvb.v, C.mla_wkv_b.v[j])
    kvg = S.sb("kvg", [128, 2], F32)
    S.dma("sp", kvg.v, C.mla_kvn_g_fm.v)
    qng = S.sb("qng", [128, 4], F32)
    S.dma("sp", qng.v, C.mla_qn_g_fm.v)
    cosb = [S.sb("pcos%d" % i, [64, 512], F32) for i in range(2)]
    sinb = [S.sb("psin%d" % i, [64, 512], F32) for i in range(2)]
    sq = [S.sb("psq%d" % i, [128, 512], F32) for i in range(2)]
    rstd = S.sb("prstd", [128, 512], F32)
    kvn = S.sb("kvn", [128, 512], BF16)
    knb = [S.sb("knb%d" % i, [128, 4, 512], BF16) for i in range(2)]
    vb = [S.sb("vb%d" % i, [128, 4, 4, 130], BF16) for i in range(2)]
    for i in range(2):
        S.memset(vb[i].v, 0.0)
        S.memset(vb[i][:, :, :, 128:129], 1.0)
    t1 = S.sb("pt1", [64, 512], F32)
    t2 = S.sb("pt2", [64, 512], F32)
    kpb = [S.sb("kpb%d" % i, [64, 512], BF16) for i in range(2)]
    qnb = [S.sb("qnb%d" % i, [128, 2, 512], BF16) for i in range(2)]
    P = [S.ps("mpP%d" % i, [128, 512], F32) for i in range(7)]
    wv = wkvb.v.re("r (h two c) -> r h two c", h=4, two=2)[:, :, 1, :]
    for bi, (t0, n) in enumerate(BLOCKS):
        ts_ = slice(t0, t0 + n)
        b2 = bi % 2
        S.dma("sp", cosb[b2][:, 0:n], C.h_cosT.v[:, ts_])
        S.dma("sp", sinb[b2][:, 0:n], C.h_sinT.v[:, ts_])
        for k in range(8):
            S.mm(P[0][:, 0:n], wm[:, k, 256:384], hT[:, k, ts_], start=(k == 0), stop=(k == 7))
        S.act(sq[0][:, 0:n], P[0][:, 0:n], AF.Square)
        S.mm(P[1][:, 0:n], C.onesf.v, sq[0][:, 0:n])
        S.act(rstd[:, 0:n], P[1][:, 0:n], AF.Sqrt, scale=1.0 / 128, bias=C.epsb.v)
        S.recip(rstd[:, 0:n], rstd[:, 0:n])
        S.stt(kvn[:, 0:n], P[0][:, 0:n], kvg[:, j:j + 1], rstd[:, 0:n], ALU.mult, ALU.mult)
        for h in range(4):
            pk = P[2 + h % 2]
            S.mm(pk[:, 0:n], wkvb[:, h * 256:h * 256 + 128], kvn[:, 0:n])
            S.cp(knb[b2][:, h, 0:n], pk[:, 0:n], eng=("act" if h % 2 else "dve"))
        S.dma("sp", C.knT_d.v[:, :, ts_], knb[b2][:, :, 0:n])
        for sub in range(n // 128):
            S.mm(P[4].v.re("p (h c) -> p h c", h=4), kvn[:, sub * 128:(sub + 1) * 128], wv)
            S.cp(vb[b2][:, sub, :, 0:128], P[4].v.re("p (h c) -> p h c", h=4))
        S.dma("sp", C.v_d.v[:, t0 // 128:t0 // 128 + n // 128, :, :], vb[b2][:, 0:n // 128, :, :])
        for k in range(8):
            S.mm(P[5][0:64, 0:n], wm[:, k, 384:448], hT[:, k, ts_], start=(k == 0), stop=(k == 7))
        for k in range(8):
            S.mm(P[6][0:64, 0:n], wmrot[:, k, :], hT[:, k, ts_], start=(k == 0), stop=(k == 7))
        S.tt(t1[:, 0:n], P[5][0:64, 0:n], cosb[b2][:, 0:n], ALU.mult)
        S.tt(t2[:, 0:n], P[6][0:64, 0:n], sinb[b2][:, 0:n], ALU.mult)
        S.tt(kpb[b2][:, 0:n], t1[:, 0:n], t2[:, 0:n], ALU.add)
        S.dma("sp", C.kpeT_d.v[:, ts_], kpb[b2][:, 0:n])
        pq = [P[0], P[2]]
        for c2 in range(2):
            for k in range(8):
                S.mm(pq[c2][:, 0:n], wm[:, k, c2 * 128:(c2 + 1) * 128], hT[:, k, ts_], start=(k == 0), stop=(k == 7))
            S.act(sq[c2][:, 0:n], pq[c2][:, 0:n], AF.Square)
        for c2 in range(2):
            S.mm(P[1][:, 0:n], C.onesf.v, sq[c2][:, 0:n], start=(c2 == 0), stop=(c2 == 1))
        S.act(rstd[:, 0:n], P[1][:, 0:n], AF.Sqrt, scale=1.0 / 256, bias=C.epsb.v)
        S.recip(rstd[:, 0:n], rstd[:, 0:n])
        for c2 in range(2):
            S.stt(qnb[b2][:, c2, 0:n], pq[c2][:, 0:n], qng[:, j * 2 + c2:j * 2 + c2 + 1], rstd[:, 0:n], ALU.mult, ALU.mult)
        S.dma("sp", C.qnT_d.v[:, :, ts_], qnb[b2][:, :, 0:n])
    S.pop()


def attention(S, C, l, ctx_out):
    j = l // 2
    S.push()
    knT = S.sb("knT", [128, 4, NTOK], BF16)
    kpeT = S.sb("kpeT", [64, NTOK], BF16)
    vall = S.sb("vall", [128, NT, 4, 130], BF16)
    qnT = S.sb("qnT", [128, 2, NTOK], BF16)
    S.dma("sp", knT.v, C.knT_d.v)
    S.dma("sp", kpeT.v, C.kpeT_d.v)
    S.dma("sp", vall.v, C.v_d.v)
    S.dma("sp", qnT.v, C.qnT_d.v)
    wqb = S.sb("wqb", [128, 2, 768], BF16)
    S.dma("pool", wqb.v, C.mla_wq_b.v[j].re("(k p) c -> p k c", p=128))
    wqrot = S.sb("wqrot", [128, 2, 4, 64], BF16)
    for kc in range(2):
        src = wqb[:, kc, :].re("p (h c) -> p h c", h=4)[:, :, 128:192].re("p h (a two s) -> p h a two s", a=2, two=2)
        dst = wqrot[:, kc, :, :].re("p h (a two s) -> p h a two s", a=2, two=2)
        for a in range(2):
            S.ts(dst[:, :, a, 0, :], src[:, :, a, 1, :], -1.0, ALU.mult)
            S.cp(dst[:, :, a, 1, :], src[:, :, a, 0, :])
    cosb = [S.sb("acos%d" % i, [64, 512], F32) for i in range(2)]
    sinb = [S.sb("asin%d" % i, [64, 512], F32) for i in range(2)]
    qhn = [S.sb("qhn%d" % i, [128, 512], BF16) for i in range(2)]
    qhp = [S.sb("qhp%d" % i, [64, 512], BF16) for i in range(2)]
    t1 = S.sb("at1", [64, 512], F32)
    t2 = S.sb("at2", [64, 512], F32)
    PT = [S.sb("aPT%d" % i, [128, 512], BF16) for i in range(2)]
    mo = [S.sb("amo%d" % i, [128, 4, 512], BF16) for i in range(2)]
    rec = S.sb("arec", [128, 4], F32)
    psS = [S.ps("apsS%d" % i, [128, 512], F32) for i in range(2)]
    psO = [S.ps("apsO%d" % i, [128, 512], F32) for i in range(4)]
    pqa = S.ps("apqa", [128, 512], F32)
    pqb = S.ps("apqb", [128, 512], F32)
    qblocks = ([(0, 256, 2)] if ctx_out else []) + [(256 + 512 * i, 512, NT) for i in range(8)]
    mv = C.mixd.v.re("(t p) f -> p t f", p=128)
    cnt = 0
    for bi, (t0, n, nk) in enumerate(qblocks):
        nq = n // 128
        ts_ = slice(t0, t0 + n)
        b2 = bi % 2
        S.dma("sp", cosb[b2][:, 0:n], C.h_cosT.v[:, ts_])
        S.dma("sp", sinb[b2][:, 0:n], C.h_sinT.v[:, ts_])
        for h in range(4):
            hh = (bi * 4 + h) % 2
            for kc in range(2):
                S.mm(pqa[:, 0:n], wqb[:, kc, h * 192:h * 192 + 128], qnT[:, kc, ts_], start=(kc == 0), stop=(kc == 1))
            S.cp(qhn[hh][:, 0:n], pqa[:, 0:n], eng="act")
            for kc in range(2):
                S.mm(pqb[0:64, 0:n], wqb[:, kc, h * 192 + 128:h * 192 + 192], qnT[:, kc, ts_], start=(kc == 0), stop=(kc == 1))
            S.tt(t1[:, 0:n], pqb[0:64, 0:n], cosb[b2][:, 0:n], ALU.mult)
            for kc in range(2):
                S.mm(pqa[0:64, 0:n], wqrot[:, kc, h, :], qnT[:, kc, ts_], start=(kc == 0), stop=(kc == 1))
            S.tt(t2[:, 0:n], pqa[0:64, 0:n], sinb[b2][:, 0:n], ALU.mult)
            S.tt(qhp[hh][:, 0:n], t1[:, 0:n], t2[:, 0:n], ALU.add)
            def emit_pv(kt_, pt_):
                for qs in range(nq):
                    S.mm(psO[qs][:, 0:129], pt_[:, qs * 128:(qs + 1) * 128], vall[:, kt_, h, 0:129],
                         start=(kt_ == 0), stop=(kt_ == nk - 1))
            pend = None
            for kt_ in range(nk):
                ps_ = psS[cnt % 2]
                pt_ = PT[cnt % 2]
                cnt += 1
                ks = slice(kt_ * 128, (kt_ + 1) * 128)
                S.mm(ps_[:, 0:n], knT[:, h, ks], qhn[hh][:, 0:n], start=True, stop=False)
                S.mm(ps_[:, 0:n], kpeT[:, ks], qhp[hh][:, 0:n], start=False, stop=True)
                if pend is not None:
                    emit_pv(*pend)
                S.act(pt_[:, 0:n], ps_[:, 0:n], AF.Exp, scale=MLA_SCALE)
                pend = (kt_, pt_)
            emit_pv(*pend)
            for qs in range(nq):
                S.recip(rec[:, qs:qs + 1], psO[qs][:, 128:129])
                S.ts(mo[b2][:, qs, h * 128:(h + 1) * 128], psO[qs][:, 0:128], rec[:, qs:qs + 1], ALU.mult)
        S.dma("sp", mv[:, t0 // 128:t0 // 128 + nq, 512:1024], mo[b2][:, 0:nq, :])
    S.pop()


def out_proj(S, C, l, ctx_out):
    j = l // 2
    S.push()
    wo = S.sb("wo", [128, 8, D], BF16)
    S.dma("pool", wo.v, C.w_out.v[j].re("(k p) d -> p k d", p=128))
    kinds = (0, 1) if ctx_out else (0,)
    G1 = {}
    for kd in kinds:
        G1[kd] = S.sb("pG1_%d" % kd, [128, D], F32)
        load_mod(S, C, l, kd, 2, G1[kd])
    mt = [S.sb("pmt%d" % i, [128, D], BF16) for i in range(2)]
    xts = [S.sb("pxt%d" % i, [128, D], F32) for i in range(2)]
    mT = [S.sb("pmT%d" % i, [128, 8, 128], BF16) for i in range(2)]
    yt = S.sb("pyt", [128, D], F32)
    xn = [S.sb("pxn%d" % i, [128, D], F32) for i in range(2)]
    pt = [S.ps("ppt%d" % i, [128, D], BF16) for i in range(2)]
    py = [S.ps("ppy%d" % i, [128, D], F32) for i in range(2)]
    tl_ = list(range(0 if ctx_out else 2, NT))

    def stage1(it):
        t = tl_[it]
        i2 = it % 2
        S.dma("sp", mt[i2].v, C.mixd.v[t * 128:(t + 1) * 128, :])
        S.dma("sp", xts[i2].v, C.xd.v[t * 128:(t + 1) * 128, :])
        for k in range(8):
            S.tr(pt[i2][:, k * 128:(k + 1) * 128], mt[i2][:, k * 128:(k + 1) * 128], C.identb.v)

    def stage2(it):
        t = tl_[it]
        kd = 1 if t < 2 else 0
        i2 = it % 2
        S.cp(mT[i2].v.re("p k t -> p (k t)"), pt[i2].v, eng="act")
        for hf in range(2):
            for k in range(8):
                S.mm(py[i2][:, hf * 512:(hf + 1) * 512], mT[i2][:, k, :], wo[:, k, hf * 512:(hf + 1) * 512],
                     start=(k == 0), stop=(k == 7))
        S.tt(yt.v, py[i2].v, G1[kd].v, ALU.mult)
        S.tt(xn[i2].v, xts[i2].v, yt.v, ALU.add)
        S.dma("pool", C.xd.v[t * 128:(t + 1) * 128, :], xn[i2].v)

    stage1(0)
    for it in range(len(tl_)):
        if it + 1 < len(tl_):
            stage1(it + 1)
        stage2(it)
    S.pop()


def even_mixer(S, C, l, ctx_out):
    S.push()
    HG = NS()
    HG.dec = S.sb("hg_dec", [128, 2, 4, 68], F32)
    HG.expm = S.sb("hg_expm", [128, 2, 4, 68], F32)
    S.push()
    hT = S.sb("hT_all", [128, 8, NTOK], BF16)
    even_E1(S, C, l, hT)
    hgrn_proj(S, C, l, hT, HG)
    mla_prep(S, C, l, hT)
    S.pop()
    hgrn_scan(S, C, l, ctx_out, HG)
    S.pop()
    attention(S, C, l, ctx_out)
    out_proj(S, C, l, ctx_out)


def build(layers=(0, 1, 2, 3), debug_out=()):
    nc = bass.Bass("TRN2", target_bir_lowering=False)
    S = Sched(nc)
    S.push()
    C = setup(S, debug_out)
    prologue(S, C, layers)
    for l in layers:
        ctx_out = l < 2
        if l % 2 == 0:
            even_mixer(S, C, l, ctx_out)
        else:
            odd_mixer(S, C, l, ctx_out)
        moe_layer(S, C, l, ctx_out)
    final_norm(S, C)
    S.pop()
    return nc, S


def make_in_maps(inp, consts):
    f = lambda a: np.ascontiguousarray(np.asarray(a, dtype=np.float32))
    shared = {
        "cctx_fm": f(np.asarray(inp["c_ctx"]).reshape(8, 128).T),
        "ada_w": f(inp["ada_w"]), "ada_b": f(inp["ada_b"]), "norm1_g": f(inp["norm1_g"]), "norm2_g": f(inp["norm2_g"]),
        "w_in": f(inp["w_in"]),
        "hg_lb_fm": f(np.asarray(inp["hg_lb"]).reshape(2, 2, 4, 128).transpose(3, 0, 1, 2).reshape(128, 16)),
        "hg_norm_g": f(inp["hg_norm_g"]),
        "mla_qn_g_fm": f(np.asarray(inp["mla_qn_g"]).reshape(2, 2, 128).transpose(2, 0, 1).reshape(128, 4)),
        "mla_wq_b": f(inp["mla_wq_b"]),
        "mla_kvn_g_fm": f(np.asarray(inp["mla_kvn_g"]).T),
        "mla_wkv_b": f(inp["mla_wkv_b"]), "w_out": f(inp["w_out"]), "pool_w": f(inp["pool_w"]),
        "pool_scale": f(inp["pool_scale"]), "router_w": f(inp["router_w"]),
        "exp_wg": f(inp["exp_wg"]), "exp_wu": f(inp["exp_wu"]), "exp_wd": f(inp["exp_wd"]),
        "final_g": f(np.asarray(inp["final_g"]).reshape(1, D)),
    }
    for k, v in consts.items():
        shared["k_" + k] = f(v).reshape(CONST_SHAPES[k])
    maps = []
    for core in range(8):
        b = core % 4
        m = dict(shared)
        m["x"] = f(inp["x"][b])
        m["ctx"] = f(inp["ctx"][b])
        m["c_fm"] = f(np.asarray(inp["c"][b]).reshape(8, 128).T)
        maps.append(m)
    return maps


def kernel(**inputs):
    nc, S = build()
    maps = make_in_maps(inputs, host_consts())
    res = run_bass_kernel_spmd(nc, maps, core_ids=list(range(8)))
    out = np.stack([np.asarray(res.results[b]["out"], dtype=np.float32) for b in range(4)], axis=0)
    return out
```
